# Optimizing a Trainium2 kernel written in Bass

```python
import jax
import jax.numpy as jnp
from jax import lax
import numpy as np

D_MODEL = 1024
BATCH = 2
SEQ = 16384
DEPTH = 2

GRID_W = 64
CTX_LEN = 256
EPS = 1e-6

N_Q_HEADS = 8
N_KV_HEADS = 2
GQA_GROUP = N_Q_HEADS // N_KV_HEADS
HEAD_DIM = 64
WINDOW = 128
ATTN_BLOCK = 128
ROPE_BASE = 10000.0
ATT_Q_W = N_Q_HEADS * HEAD_DIM
ATT_KV_W = N_KV_HEADS * HEAD_DIM
SGU_HEADS = 4
SGU_HEAD_DIM = 64
SGU_W = SGU_HEADS * SGU_HEAD_DIM
SGU_CHUNK = 128
POOL_WINDOWS = (2, 4, 8, 16)
POOL_GROUPS = 4
POOL_GROUP_DIM = 64
POOL_CH = POOL_GROUPS * POOL_GROUP_DIM
CONV_CH = 256
CONV_WIDTH = 3

MIX_WIDTH = ATT_Q_W + SGU_W + POOL_CH + CONV_CH
IN_COLS = ATT_Q_W + 2 * ATT_KV_W + 2 * SGU_W + POOL_CH + 3 * CONV_CH
_Q_END = ATT_Q_W
_K_END = _Q_END + ATT_KV_W
_V_END = _K_END + ATT_KV_W
_SU_END = _V_END + SGU_W
_SV_END = _SU_END + SGU_W
_POOL_END = _SV_END + POOL_CH
_CB_END = _POOL_END + CONV_CH
_CC_END = _CB_END + CONV_CH
SPLIT_POINTS = (_Q_END, _K_END, _V_END, _SU_END, _SV_END, _POOL_END, _CB_END, _CC_END)

N_EXPERTS = 32
TOP_K = 4
D_FF_EXPERT = 1024
SWIGLU_LIMIT = 7.0
SWIGLU_ALPHA = 1.702
MOE_BLOCK = 128

kernel_name = 'hybrid_parallel_group_dit_moe'


def rms_norm(x, g):
    xf = x.astype(jnp.float32)
    y = xf * lax.rsqrt(jnp.mean(xf * xf, axis=-1, keepdims=True) + EPS)
    return (y * g.astype(jnp.float32)).astype(x.dtype)


def layer_norm(x, g, b):
    xf = x.astype(jnp.float32)
    mu = jnp.mean(xf, axis=-1, keepdims=True)
    var = jnp.mean(jnp.square(xf - mu), axis=-1, keepdims=True)
    y = (xf - mu) * lax.rsqrt(var + EPS) * g.astype(jnp.float32) + b.astype(jnp.float32)
    return y.astype(x.dtype)


def modulate(h, shift, scale):
    return h * (1 + scale) + shift


def heads(t, n):
    return t.reshape(t.shape[:-1] + (n, t.shape[-1] // n))


def axial_rope_tables(n_tokens):
    rows = n_tokens // GRID_W
    row = jnp.repeat(jnp.arange(rows), GRID_W).astype(jnp.float32)
    col = jnp.tile(jnp.arange(GRID_W), rows).astype(jnp.float32)
    n_freq = HEAD_DIM // 4
    inv = ROPE_BASE ** (-jnp.arange(n_freq, dtype=jnp.float32) / n_freq)
    ang = jnp.concatenate([row[:, None] * inv, col[:, None] * inv], axis=-1)
    return jnp.cos(ang), jnp.sin(ang)


def apply_rope(x, cos, sin):
    xf = x.astype(jnp.float32)
    half = HEAD_DIM // 2
    x1, x2 = xf[..., :half], xf[..., half:]
    cs, sn = cos[None, :, None, :], sin[None, :, None, :]
    return jnp.concatenate([x1 * cs - x2 * sn, x1 * sn + x2 * cs], axis=-1).astype(x.dtype)


def window_attention(q, k, v, kc, vc, sink):
    B, S = q.shape[0], q.shape[1]
    L = kc.shape[1]
    nb = S // ATTN_BLOCK
    scale = HEAD_DIM ** -0.5
    qb = q.reshape(B, nb, ATTN_BLOCK, N_KV_HEADS, GQA_GROUP, HEAD_DIM)

    def band(t):
        tp = jnp.pad(t, ((0, 0), (ATTN_BLOCK, ATTN_BLOCK), (0, 0), (0, 0)))
        blocks = tp.reshape(B, nb + 2, ATTN_BLOCK, N_KV_HEADS, HEAD_DIM)
        return jnp.concatenate([blocks[:, :-2], blocks[:, 1:-1], blocks[:, 2:]], axis=2)

    kb, vb = band(k), band(v)
    blk = jnp.arange(nb)[:, None, None]
    qpos = blk * ATTN_BLOCK + jnp.arange(ATTN_BLOCK)[None, :, None]
    kpos = (blk - 1) * ATTN_BLOCK + jnp.arange(3 * ATTN_BLOCK)[None, None, :]
    valid = (jnp.abs(qpos - kpos) <= WINDOW) & (kpos >= 0) & (kpos < S)
    s_band = jnp.einsum('bnqkgd,bnskd->bnkgqs', qb, kb).astype(jnp.float32) * scale
    s_band = jnp.where(valid[None, :, None, None], s_band, -jnp.inf)
    s_ctx = jnp.einsum('bnqkgd,blkd->bnkgql', qb, kc).astype(jnp.float32) * scale
    s_sink = jnp.broadcast_to(sink.astype(jnp.float32).reshape(N_KV_HEADS, GQA_GROUP)[None, None, :, :, None, None],
                              s_band.shape[:-1] + (1,))
    probs = jax.nn.softmax(jnp.concatenate([s_band, s_ctx, s_sink], axis=-1), axis=-1)
    nband = 3 * ATTN_BLOCK
    p_band = probs[..., :nband].astype(v.dtype)
    p_ctx = probs[..., nband:nband + L].astype(vc.dtype)
    o = (jnp.einsum('bnkgqs,bnskd->bnqkgd', p_band, vb)
         + jnp.einsum('bnkgql,blkd->bnqkgd', p_ctx, vc))
    return o.reshape(B, S, ATT_Q_W)


def context_attention(qc, kc, vc, sink):
    B, L = qc.shape[0], qc.shape[1]
    scale = HEAD_DIM ** -0.5
    qg = qc.reshape(B, L, N_KV_HEADS, GQA_GROUP, HEAD_DIM)
    s = jnp.einsum('bqkgd,blkd->bkgql', qg, kc).astype(jnp.float32) * scale
    s_sink = jnp.broadcast_to(sink.astype(jnp.float32).reshape(N_KV_HEADS, GQA_GROUP)[None, :, :, None, None],
                              s.shape[:-1] + (1,))
    probs = jax.nn.softmax(jnp.concatenate([s, s_sink], axis=-1), axis=-1)[..., :L]
    o = jnp.einsum('bkgql,blkd->bqkgd', probs.astype(vc.dtype), vc)
    return o.reshape(B, L, ATT_Q_W)


def chunk_sgu(u, v, ws, bs, ln_g, ln_b):
    B, N, _ = v.shape
    nc = N // SGU_CHUNK
    vn = layer_norm(v, ln_g, ln_b).reshape(B, nc, SGU_CHUNK, SGU_HEADS, SGU_HEAD_DIM)
    s = jnp.einsum('hpr,bcrhd->bcphd', ws, vn) + bs.T[None, None, :, :, None]
    return u * s.reshape(B, N, SGU_W)


def multiscale_pool(xp, pool_w, pool_scale):
    B, N, _ = xp.shape
    xf = xp.astype(jnp.float32)
    cs = jnp.concatenate([jnp.zeros((B, 1, POOL_CH), jnp.float32), jnp.cumsum(xf, axis=1)], axis=1)
    t = jnp.arange(N)
    pooled = []
    for gi, w in enumerate(POOL_WINDOWS):
        lo = jnp.clip(t - w // 2, 0, N)
        hi = jnp.clip(t + w - w // 2, 0, N)
        csg = cs[..., gi * POOL_GROUP_DIM:(gi + 1) * POOL_GROUP_DIM]
        cnt = (hi - lo).astype(jnp.float32)
        pooled.append((csg[:, hi] - csg[:, lo]) / cnt[None, :, None])
    d = (jnp.concatenate(pooled, axis=-1) - xf).astype(xp.dtype)
    d = d.reshape(B, N, POOL_GROUPS, POOL_GROUP_DIM)
    y = jnp.einsum('bngc,gcd->bngd', d, pool_w).reshape(B, N, POOL_CH)
    return y * pool_scale


def short_conv(cb, cc, cx, conv_w):
    y = cc * cx
    rhs = conv_w[:, None, :].astype(y.dtype)
    z = lax.conv_general_dilated(y, rhs, window_strides=(1,),
                                 padding=((CONV_WIDTH // 2, CONV_WIDTH // 2),),
                                 dimension_numbers=('NWC', 'WIO', 'NWC'),
                                 feature_group_count=CONV_CH)
    return cb * z


def gated_mixers(su, sv, xp, cb, cc, cx, sgu_ws, sgu_b, sgu_ln_g, sgu_ln_b, pool_w, pool_scale, conv_w):
    y_b = chunk_sgu(jax.nn.gelu(su, approximate=False), jax.nn.gelu(sv, approximate=False),
                    sgu_ws, sgu_b, sgu_ln_g, sgu_ln_b)
    y_c = multiscale_pool(xp, pool_w, pool_scale)
    y_d = short_conv(cb, cc, cx, conv_w)
    return y_b, y_c, y_d


def moe(h, router_w, router_b, w1, b1, w2, b2):
    T, D = h.shape
    logits = (h @ router_w).astype(jnp.float32) + router_b.astype(jnp.float32)
    top_v, top_i = lax.top_k(logits, TOP_K)
    gates = jax.nn.softmax(top_v, axis=-1)
    flat_e = top_i.reshape(-1)
    flat_t = jnp.broadcast_to(jnp.arange(T)[:, None], (T, TOP_K)).reshape(-1)
    flat_g = gates.reshape(-1)
    order = jnp.argsort(flat_e)
    se, st, sg = flat_e[order], flat_t[order], flat_g[order]
    counts = jnp.bincount(flat_e, length=N_EXPERTS)
    starts = jnp.cumsum(counts) - counts
    padded = (counts + MOE_BLOCK - 1) // MOE_BLOCK * MOE_BLOCK
    pends = jnp.cumsum(padded)
    pstarts = pends - padded
    dest = pstarts[se] + (jnp.arange(T * TOP_K) - starts[se])
    n_blocks = -(-(T * TOP_K) // MOE_BLOCK) + N_EXPERTS
    n_slots = n_blocks * MOE_BLOCK
    slot_tok = jnp.full((n_slots,), T, jnp.int32).at[dest].set(st.astype(jnp.int32))
    slot_gate = jnp.zeros((n_slots,), h.dtype).at[dest].set(sg.astype(h.dtype))
    block_e = jnp.minimum(jnp.searchsorted(pends, jnp.arange(n_blocks) * MOE_BLOCK, side='right'), N_EXPERTS - 1)
    h_pad = jnp.concatenate([h, jnp.zeros((1, D), h.dtype)], axis=0)
    xs = h_pad[slot_tok].reshape(n_blocks, MOE_BLOCK, D)

    def expert_block(args):
        xb, e = args
        z = xb @ w1[e] + b1[e]
        g = jnp.minimum(z[:, ::2], SWIGLU_LIMIT)
        lin = jnp.clip(z[:, 1::2], -SWIGLU_LIMIT, SWIGLU_LIMIT)
        a = g * jax.nn.sigmoid(SWIGLU_ALPHA * g) * (lin + 1)
        return a @ w2[e] + b2[e]

    ys = lax.map(expert_block, (xs, block_e)).reshape(n_slots, D)
    out = jnp.zeros((T + 1, D), h.dtype).at[slot_tok].add(ys * slot_gate[:, None])
    return out[:T]


def setup_inputs(seed: int = 0) -> dict:
    key = jax.random.key(seed)
    ks = jax.random.split(key, 26)
    f32 = jnp.float32
    D = D_MODEL
    L = DEPTH

    def nrm(k, shape, s):
        return jax.random.normal(k, shape, f32) * s

    return {
        'x': nrm(ks[0], (BATCH, SEQ, D), 1.0),
        'c': nrm(ks[1], (BATCH, D), 1.0),
        'ctx': nrm(ks[2], (BATCH, CTX_LEN, D), 1.0),
        'c_ctx': nrm(ks[3], (D,), 1.0),
        'norm1_g': 1.0 + nrm(ks[4], (L, D), 0.02),
        'norm2_g': 1.0 + nrm(ks[5], (L, D), 0.02),
        'ada_w': nrm(ks[6], (L, D, 6 * D), 0.5 * D ** -0.5),
        'ada_b': nrm(ks[7], (L, 6 * D), 0.02),
        'w_in': nrm(ks[8], (L, D, IN_COLS), D ** -0.5),
        'attn_sink': nrm(ks[9], (L, N_Q_HEADS), 0.5),
        'sgu_ws': nrm(ks[10], (L, SGU_HEADS, SGU_CHUNK, SGU_CHUNK), 0.5 * SGU_CHUNK ** -0.5),
        'sgu_b': 1.0 + nrm(ks[11], (L, SGU_HEADS, SGU_CHUNK), 0.02),
        'sgu_ln_g': 1.0 + nrm(ks[12], (L, SGU_W), 0.02),
        'sgu_ln_b': nrm(ks[13], (L, SGU_W), 0.02),
        'pool_w': nrm(ks[14], (L, POOL_GROUPS, POOL_GROUP_DIM, POOL_GROUP_DIM), POOL_GROUP_DIM ** -0.5),
        'pool_scale': 1.0 + nrm(ks[15], (L, POOL_CH), 0.02),
        'conv_w': nrm(ks[16], (L, CONV_WIDTH, CONV_CH), CONV_WIDTH ** -0.5),
        'w_out': nrm(ks[17], (L, MIX_WIDTH, D), MIX_WIDTH ** -0.5),
        'router_w': nrm(ks[18], (L, D, N_EXPERTS), D ** -0.5),
        'router_b': nrm(ks[19], (L, N_EXPERTS), 0.01),
        'exp_w1': nrm(ks[20], (L, N_EXPERTS, D, 2 * D_FF_EXPERT), D ** -0.5),
        'exp_b1': nrm(ks[21], (L, N_EXPERTS, 2 * D_FF_EXPERT), 0.02),
        'exp_w2': nrm(ks[22], (L, N_EXPERTS, D_FF_EXPERT, D), D_FF_EXPERT ** -0.5),
        'exp_b2': nrm(ks[23], (L, N_EXPERTS, D), 0.02),
        'final_g': 1.0 + nrm(ks[24], (D,), 0.02),
    }


def reference(x, c, ctx, c_ctx, norm1_g, norm2_g, ada_w, ada_b, w_in, attn_sink, sgu_ws, sgu_b,
              sgu_ln_g, sgu_ln_b, pool_w, pool_scale, conv_w, w_out, router_w, router_b,
              exp_w1, exp_b1, exp_w2, exp_b2, final_g):
    B, S, D = x.shape
    L = ctx.shape[1]
    cos, sin = axial_rope_tables(S)
    xl, xc = x, ctx
    silu_c = jax.nn.silu(c)
    silu_cc = jax.nn.silu(c_ctx)
    for l in range(DEPTH):
        last = l == DEPTH - 1
        mod_l = (silu_c @ ada_w[l] + ada_b[l])[:, None, :]
        mod_c = (silu_cc @ ada_w[l] + ada_b[l])[None, None, :]
        sh1l, sc1l, g1l, sh2l, sc2l, g2l = jnp.split(mod_l, 6, axis=-1)
        sh1c, sc1c, g1c, sh2c, sc2c, g2c = jnp.split(mod_c, 6, axis=-1)
        mix_params = (sgu_ws[l], sgu_b[l], sgu_ln_g[l], sgu_ln_b[l], pool_w[l], pool_scale[l], conv_w[l])

        hc = modulate(rms_norm(xc, norm1_g[l]), sh1c, sc1c)
        if last:
            kv_c = hc @ w_in[l][:, _Q_END:_V_END]
            kc = heads(kv_c[..., :ATT_KV_W], N_KV_HEADS)
            vc = heads(kv_c[..., ATT_KV_W:], N_KV_HEADS)
        else:
            pc = jnp.split(hc @ w_in[l], SPLIT_POINTS, axis=-1)
            kc = heads(pc[1], N_KV_HEADS)
            vc = heads(pc[2], N_KV_HEADS)
            attn_c = context_attention(heads(pc[0], N_Q_HEADS), kc, vc, attn_sink[l])
            yb_c, yc_c, yd_c = gated_mixers(*pc[3:], *mix_params)
            mix_c = jnp.concatenate([attn_c, yb_c, yc_c, yd_c], axis=-1) @ w_out[l]
            xc_mid = xc + g1c * mix_c

        hl = modulate(rms_norm(xl, norm1_g[l]), sh1l, sc1l)
        pl = jnp.split(hl @ w_in[l], SPLIT_POINTS, axis=-1)
        ql = apply_rope(heads(pl[0], N_Q_HEADS), cos, sin)
        kl = apply_rope(heads(pl[1], N_KV_HEADS), cos, sin)
        vl = heads(pl[2], N_KV_HEADS)
        attn_l = window_attention(ql, kl, vl, kc, vc, attn_sink[l])
        yb_l, yc_l, yd_l = gated_mixers(*pl[3:], *mix_params)
        mix_l = jnp.concatenate([attn_l, yb_l, yc_l, yd_l], axis=-1) @ w_out[l]
        xl = xl + g1l * mix_l

        hl2 = modulate(rms_norm(xl, norm2_g[l]), sh2l, sc2l).reshape(B * S, D)
        moe_args = (router_w[l], router_b[l], exp_w1[l], exp_b1[l], exp_w2[l], exp_b2[l])
        if last:
            xl = xl + g2l * moe(hl2, *moe_args).reshape(B, S, D)
        else:
            hc2 = modulate(rms_norm(xc_mid, norm2_g[l]), sh2c, sc2c).reshape(B * L, D)
            y = moe(jnp.concatenate([hl2, hc2], axis=0), *moe_args)
            xl = xl + g2l * y[:B * S].reshape(B, S, D)
            xc = xc_mid + g2c * y[B * S:].reshape(B, L, D)
    return rms_norm(xl, final_g)
```

```python
import numpy as np
from contextlib import ExitStack
import concourse.bass as bass
import concourse.mybir as mybir

F32 = mybir.dt.float32
BF16 = mybir.dt.bfloat16
AF = mybir.ActivationFunctionType
ALU = mybir.AluOpType
AX = mybir.AxisListType


class Emit:
    NDSEM = 10

    def __init__(self, nc, es, needed=None):
        self.nc = nc
        self.needed = needed
        self.collected = set()
        self.last_inc = {k: 0 for k in ("pe", "act", "dve", "pool")}
        self.eng = {"pe": nc.tensor, "act": nc.scalar, "dve": nc.vector,
                    "pool": nc.gpsimd, "sp": nc.sync}
        self.esem = {k: es.enter_context(nc.semaphore("e_" + k))
                     for k in ("pe", "act", "dve", "pool")}
        self.ecnt = {k: 0 for k in self.esem}
        self.dsem = {}
        self.dcnt = {}
        self.dnext = {}
        for q in ("sp", "act", "pool"):
            self.dsem[q] = [es.enter_context(nc.semaphore(f"d_{q}{i}"))
                            for i in range(self.NDSEM)]
            self.dnext[q] = 0
            for i in range(self.NDSEM):
                self.dcnt[(q, i)] = 0
        self.seen = {k: {} for k in self.eng}
        self.bufs = {}
        self.nwaits = 0
        self.nins = 0

    def _sem(self, key):
        if isinstance(key, tuple):
            return self.dsem[key[0]][key[1]]
        return self.esem[key]

    def _deps(self, reads, writes):
        deps = {}
        for k in reads:
            st = self.bufs.get(k)
            if st and st["w"]:
                s, v = st["w"]
                deps[s] = max(deps.get(s, 0), v)
        for k in writes:
            st = self.bufs.get(k)
            if st:
                if st["w"]:
                    s, v = st["w"]
                    deps[s] = max(deps.get(s, 0), v)
                for s, v in st["r"].items():
                    deps[s] = max(deps.get(s, 0), v)
        return deps

    def _wait(self, e, deps):
        seen = self.seen[e]
        for s, v in deps.items():
            if e == "pe" and s == "pe":
                continue
            if not isinstance(s, tuple):
                self.collected.add((s, v))
            if seen.get(s, 0) >= v:
                continue
            self.eng[e].wait_ge(self._sem(s), v)
            if not isinstance(s, tuple):
                self.collected.add((s, v))
            seen[s] = v
            self.nwaits += 1

    def _record(self, tok, reads, writes):
        s, v = tok
        for k in reads:
            st = self.bufs.setdefault(k, {"w": None, "r": {}})
            st["r"][s] = max(st["r"].get(s, 0), v)
        for k in writes:
            st = self.bufs.setdefault(k, {"w": None, "r": {}})
            st["w"] = tok
            st["r"] = {}

    dead = False

    def op(self, e, fn, reads=(), writes=()):
        if self.dead:
            return None
        self._wait(e, self._deps(reads, writes))
        ins = fn(self.eng[e])
        self.ecnt[e] += 1
        c = self.ecnt[e]
        if self.needed is None or (e, c) in self.needed:
            ins.then_inc(self.esem[e], c - self.last_inc[e])
            self.last_inc[e] = c
        self._record((e, c), reads, writes)
        self.nins += 1
        return ins

    def dma(self, q, out, in_, reads=(), writes=(), **kw):
        if self.dead:
            return None
        i = self.dnext[q]
        self.dnext[q] = (i + 1) % self.NDSEM
        key = (q, i)
        deps = self._deps(reads, writes)
        deps[key] = max(deps.get(key, 0), self.dcnt[key])
        self._wait(q, deps)
        ins = self.eng[q].dma_start(out=out, in_=in_, **kw)
        self.dcnt[key] += 16
        ins.then_inc(self.dsem[q][i], 16)
        self._record((key, self.dcnt[key]), reads, writes)
        self.nins += 1
        return ins

    def wait_all(self, e, keys):
        deps = self._deps(keys, ())
        self._wait(e, deps)

    def barrier(self):
        if self.dead:
            return
        deps = {k: self.ecnt[k] for k in self.esem}
        deps.update({k: v for k, v in self.dcnt.items()})
        for e in self.eng:
            self._wait(e, dict(deps))

from concourse.bass_utils import run_bass_kernel_spmd

NT = 36
D = 1024
EPS = 1e-6
C_Q, C_QR, C_K, C_KR, C_SU, C_PO, C_CB, C_CC, C_CX, C_VSV = 0, 512, 1024, 1152, 1280, 1536, 1792, 2048, 2304, 2560
NWC = 2944


class _Stop(Exception):
    pass


def build(nc, nlayers=2, dbg=False, nexp=32, stop=0, nblk=99, needed=None):
    es = ExitStack()
    E = Emit(nc, es, needed)

    def din(name, shape, dt=F32):
        return nc.dram_tensor(name, list(shape), dt, kind="ExternalInput").ap()

    xs = din("xs", [NT * 128, D]); ctxb = din("ctxb", [256, D]); cvec = din("cvec", [128, 16])
    rope = din("rope", [2, 128, NT * 128]); masks = din("masks", [2, 128, 128]); flags = din("flags", [128, 4])
    ptab = din("ptab", [128, 5, 2, 128])
    norm1_g = din("norm1_g", [2, D]); norm2_g = din("norm2_g", [2, D])
    ada_w = din("ada_w", [2, D, 6 * D]); ada_b = din("ada_b", [2, 6 * D])
    w_in = din("w_in", [2, D, 2304]); attn_sink = din("attn_sink", [2, 8])
    sgu_ws = din("sgu_ws", [2, 4, 128, 128]); sgu_b = din("sgu_b", [2, 4, 128])
    sgu_ln_g = din("sgu_ln_g", [2, 256]); sgu_ln_b = din("sgu_ln_b", [2, 256])
    pool_w = din("pool_w", [2, 4, 64, 64]); pool_scale = din("pool_scale", [2, 256])
    conv_w = din("conv_w", [2, 3, 256]); w_out = din("w_out", [2, 1280, D])
    router_w = din("router_w", [2, D, 32]); router_b = din("router_b", [2, 32])
    exp_w1 = din("exp_w1", [2, 32, D, 2048]); exp_b1 = din("exp_b1", [2, 32, 2048])
    exp_w2 = din("exp_w2", [2, 32, D, D]); exp_b2 = din("exp_b2", [2, 32, D])
    final_g = din("final_g", [D])
    yout = nc.dram_tensor("y", [32 * 128, D], F32, kind="ExternalOutput").ap()
    xm_s = nc.dram_tensor("xm_s", [NT * 128, D], F32, kind="ExternalOutput" if dbg else "Internal").ap()
    x1_s = nc.dram_tensor("x1_s", [NT * 128, D], F32, kind="Internal").ap()
    xcm_s = nc.dram_tensor("xcm_s", [256, D], F32, kind="Internal").ap()
    xc1_s = nc.dram_tensor("xc1_s", [256, D], F32, kind="Internal").ap()

    def SB(st, name, shape, dt=F32):
        return st.enter_context(nc.sbuf_tensor(name, list(shape), dt))

    def PS(name, shape, dt=F32):
        return es.enter_context(nc.psum_tensor(name, list(shape), dt))

    pA = PS("pA", [128, 512]); pB = PS("pB", [128, 512]); pC = PS("pC", [128, 512])
    pT = PS("pT", [128, 8, 128], BF16)
    pO = PS("pO", [128, 512]); pDen = PS("pDen", [128, 512])
    pW = [PS("pW0", [128, 512]), PS("pW1", [128, 512])]
    PN = {id(pA): "pA", id(pB): "pB", id(pC): "pC", id(pO): "pO", id(pDen): "pDen"}

    G = es
    ident_f = SB(G, "ident_f", [128, 128]); ident_b = SB(G, "ident_b", [128, 128], BF16)
    ones_f = SB(G, "ones_f", [128, 128]); ones_b = SB(G, "ones_b", [128, 128], BF16)
    mk = SB(G, "mk", [128, 4, 128], BF16)
    mkf = SB(G, "mkf", [128, 2, 128])
    flg = SB(G, "flg", [128, 4]); ptb = SB(G, "ptb", [128, 5, 2, 128])
    csv = SB(G, "csv", [128, 16])

    E.op("pool", lambda e: e.memset(ident_f[:], 1.0), writes=["ident_f"])
    E.op("pool", lambda e: e.affine_select(out=ident_f[:], in_=ident_f[:], pattern=[[-1, 128]],
                                           compare_op=ALU.is_equal, fill=0.0, base=0, channel_multiplier=1),
         reads=["ident_f"], writes=["ident_f"])
    E.op("pool", lambda e: e.tensor_copy(out=ident_b[:], in_=ident_f[:]), reads=["ident_f"], writes=["ident_b"])
    E.op("pool", lambda e: e.memset(ones_f[:], 1.0), writes=["ones_f"])
    E.op("pool", lambda e: e.memset(ones_b[:], 1.0), writes=["ones_b"])
    E.dma("sp", mkf[:], masks.rearrange("m p q -> p m q"), writes=["mkf"])
    E.dma("sp", flg[:], flags[:, :], writes=["flg"])
    E.dma("sp", ptb[:], ptab[:, :, :, :], writes=["ptb"])
    E.dma("sp", csv[:], cvec[:, :], writes=["csv"])
    E.op("dve", lambda e: e.tensor_copy(out=mk[:, 0:2, :], in_=mkf[:]), reads=["mkf"], writes=["mk"])
    E.op("dve", lambda e: e.tensor_scalar(out=mk[:, 2, :], in0=mkf[:, 0, :], scalar1=flg[:, 1:2], scalar2=None, op0=ALU.mult),
         reads=["mkf", "flg"], writes=["mk"])
    E.op("dve", lambda e: e.tensor_scalar(out=mk[:, 3, :], in0=mkf[:, 1, :], scalar1=flg[:, 2:3], scalar2=None, op0=ALU.mult),
         reads=["mkf", "flg"], writes=["mk"])
    E.op("act", lambda e: e.activation(out=csv[:], in_=csv[:], func=AF.Silu), reads=["csv"], writes=["csv"])

    uid = [0]

    def nm(s):
        uid[0] += 1
        return f"{s}_{uid[0]}"

    def bcast_load(st, dst, src_vec, key):
        E.dma("sp", dst, src_vec.partition_broadcast(128), writes=[key])

    def compute_mod(st, l, half, which, modT, tmp_stage, skey, csb, ckey2):
        for kc in range(8):
            E.op("pool", lambda e: e.tensor_copy(out=csb[:, kc, :], in_=csv[:, which * 8 + kc: which * 8 + kc + 1].to_broadcast([128, 128])),
                 reads=["csv"], writes=[ckey2])
        for nb in range(24):
            col0 = half * 3072 + nb * 128
            E.dma("sp", tmp_stage, ada_w[l, :, col0:col0 + 128].rearrange("(c p) n -> p c n", p=128), writes=[skey])
            sl = modT[:, nb // 8, (nb % 8) * 128:(nb % 8) * 128 + 128]
            bcast_load(st, sl, ada_b[l, col0:col0 + 128], "modT")
            for kc in range(8):
                E.op("pe", lambda e: e.matmul(pA[:, 0:128], lhsT=csb[:, kc, :], rhs=tmp_stage[:, kc, :], start=(kc == 0), stop=(kc == 7)),
                     reads=[ckey2, skey], writes=["pA"])
            E.op("dve", lambda e: e.tensor_tensor(out=sl, in0=pA[:, 0:128], in1=sl, op=ALU.add), reads=["pA", "modT"], writes=["modT"])

    def finish_mod(st, l, normg, modT, gts):
        for hf, (gap, gk) in enumerate(gts):
            bcast_load(st, gap, normg[l, hf * 512:(hf + 1) * 512], gk)
            E.op("dve", lambda e: e.scalar_tensor_tensor(out=modT[:, 1, hf * 512:(hf + 1) * 512], in0=modT[:, 1, hf * 512:(hf + 1) * 512], scalar=1.0, in1=gap, op0=ALU.add, op1=ALU.mult),
                 reads=["modT", gk], writes=["modT"])

    def norm_mod(xt, xkey, modT, out, okey, st_small, tmp, tmpkey):
        ssq, rt = st_small
        E.op("pool", lambda e: e.memset(ssq[:], 0.0), writes=["ssq"])
        E.op("act", lambda e: e.activation(out=tmp[:], in_=xt, func=AF.Square, accum_out=ssq[:]), reads=[xkey], writes=[tmpkey, "ssq"])
        E.op("act", lambda e: e.activation(out=rt[:], in_=ssq[:], func=AF.Sqrt, scale=1.0 / D, bias=EPS), reads=["ssq"], writes=["rt"])
        E.op("dve", lambda e: e.reciprocal(out=rt[:], in_=rt[:]), reads=["rt"], writes=["rt"])
        E.op("dve", lambda e: e.scalar_tensor_tensor(out=tmp[:], in0=xt, scalar=rt[:, 0:1], in1=modT[:, 1, :], op0=ALU.mult, op1=ALU.mult),
             reads=[xkey, "rt", "modT"], writes=[tmpkey])
        E.op("pool", lambda e: e.tensor_tensor(out=out, in0=tmp[:], in1=modT[:, 0, :], op=ALU.add), reads=[tmpkey, "modT"], writes=[okey])

    def chk(k):
        if stop == k:
            E.dead = True

    try:
        for l in range(nlayers):
            last = (l == nlayers - 1)
            xsrc = xs if l == 0 else x1_s
            csrc = ctxb if l == 0 else xc1_s
            p_lo, p_hi = (0, NT) if l == 0 else (1, NT - 1)
            m_lo, m_hi = (1, NT - 1) if l == 0 else (2, NT - 2)
            chk(1)
            E.barrier()
            with ExitStack() as ph:
                modT = SB(ph, nm("modT"), [128, 3, D])
                winb = SB(ph, nm("winb"), [128, 8, NWC], BF16)
                woA = SB(ph, nm("woA"), [64, 8, D], BF16); woB = SB(ph, nm("woB"), [128, 6, D], BF16)
                wsT = SB(ph, nm("wsT"), [128, 4, 128], BF16); bsT = SB(ph, nm("bsT"), [128, 2, 128])
                lng = SB(ph, nm("lng"), [128, 256]); lnb = SB(ph, nm("lnb"), [128, 256])
                PWm = SB(ph, nm("PW"), [128, 2, 128], BF16); PWf = SB(ph, nm("PWf"), [128, 2, 128])
                pscale = SB(ph, nm("pscale"), [128, 2]); cw = SB(ph, nm("cw"), [128, 2, 3])
                es8 = SB(ph, nm("es8"), [128, 8]); esink = SB(ph, nm("esink"), [64, 8, 128])
                kT = SB(ph, nm("kT"), [128, 3, 512], BF16); Vt = SB(ph, nm("V"), [128, 3, 4, 128], BF16)
                xpT = SB(ph, nm("xpT"), [128, 3, 2, 512], BF16); yT = SB(ph, nm("yT"), [128, 3, 2, 512], BF16)
                kcT = SB(ph, nm("kcT"), [128, 256], BF16); Vc = SB(ph, nm("Vc"), [128, 2, 128], BF16)
                qT = SB(ph, nm("qT"), [128, 2, 4, 512], BF16); suT = SB(ph, nm("suT"), [128, 2, 2, 512], BF16)
                cbT = SB(ph, nm("cbT"), [128, 2, 2, 512], BF16); ybT = SB(ph, nm("ybT"), [128, 2, 2, 512], BF16)
                ssq = SB(ph, nm("ssq"), [128, 1]); rt = SB(ph, nm("rt"), [128, 1])
                s1 = SB(ph, nm("s1"), [128, 1]); s2 = SB(ph, nm("s2"), [128, 1]); s3 = SB(ph, nm("s3"), [128, 1])
                tp = ph
                if True:
                    xt = SB(tp, nm("xt"), [128, D]); tmpf = SB(tp, nm("tmpf"), [128, D]); hb = SB(tp, nm("hb"), [128, D], BF16)
                    hT = SB(tp, nm("hT"), [128, 8, 512], BF16)
                    xr = SB(tp, nm("xr"), [128, D]); tmpM = SB(tp, nm("tmpM"), [128, D])
                    cs_t = SB(tp, nm("cs"), [128, 2, 512]); sn_t = SB(tp, nm("sn"), [128, 2, 512])
                    t1 = SB(tp, nm("t1"), [128, 512]); t2 = SB(tp, nm("t2"), [128, 512])
                    svg = SB(tp, nm("svg"), [128, 256]); vn = SB(tp, nm("vn"), [128, 256]); vnz = SB(tp, nm("vnz"), [128, 4, 128], BF16)
                    pexp = SB(tp, nm("pexp"), [128, 2, 512], BF16)
                    den = SB(tp, nm("den"), [64, 512]); attnT = SB(tp, nm("attnT"), [64, 2, 4, 128], BF16)
                    Xw = SB(tp, nm("Xw"), [128, 2, 160]); A2 = SB(tp, nm("A2"), [128, 2, 160]); A4 = SB(tp, nm("A4"), [128, 2, 160])
                    A8 = SB(tp, nm("A8"), [128, 2, 160]); A16 = SB(tp, nm("A16"), [128, 2, 160]); pd = SB(tp, nm("pd"), [128, 2, 128])
                    dT = SB(tp, nm("dT"), [128, 2, 128], BF16); ycT = SB(tp, nm("ycT"), [128, 2, 128], BF16)
                    Yw = SB(tp, nm("Yw"), [128, 2, 130]); zc = SB(tp, nm("zc"), [128, 2, 128]); ydT = SB(tp, nm("ydT"), [128, 2, 128], BF16)
                stage_ap = tmpf[:].rearrange("p (c n) -> p c n", c=8)
                csb_ap = xt[:].rearrange("p (c n) -> p c n", c=8)
                gts = [(t1[:], "t1"), (t2[:], "t2")]
                E.op("pool", lambda e: e.memset(cs_t[:, 1, :], 1.0), writes=[("cs", 1)])
                E.op("pool", lambda e: e.memset(sn_t[:, 1, :], 0.0), writes=[("cs", 1)])

                with ExitStack() as sg:
                    stg = SB(sg, nm("stg"), [128, 1, 1152])
                    for kc in range(8):
                        wk = ("winb", kc)
                        wb_ = winb[:, kc, :]
                        sk = ("stg", 0)
                        s_ = stg[:, 0, :]
                        E.dma("sp", s_, w_in[l, kc * 128:(kc + 1) * 128, 1152:2304], writes=[sk])
                        E.op("dve", lambda e: e.tensor_copy(out=wb_[:, C_PO:C_PO + 1024], in_=s_[:, 128:1152]), reads=[sk], writes=[wk])
                        E.op("pool", lambda e: e.tensor_copy(out=wb_[:, C_VSV + 256:C_VSV + 384], in_=s_[:, 0:128]), reads=[sk], writes=[wk])
                        sk = ("stg", 0)
                        s_ = stg[:, 0, :]
                        E.dma("sp", s_, w_in[l, kc * 128:(kc + 1) * 128, 0:1152], writes=[sk])
                        qv = s_[:, 0:512].rearrange("p (a j d) -> p j a d", a=2, j=4)
                        E.op("pool", lambda e: e.tensor_copy(out=wb_[:, C_Q:C_Q + 512].rearrange("p (j a d) -> p j a d", j=4, a=2), in_=qv),
                             reads=[sk], writes=[wk])
                        qvh = s_[:, 0:512].rearrange("p (a j t d) -> p j a t d", a=2, j=4, t=2)
                        qro = wb_[:, C_QR:C_QR + 512].rearrange("p (j a t d) -> p j a t d", j=4, a=2, t=2)
                        E.op("act", lambda e: e.mul(out=qro[:, :, :, 0, :], in_=qvh[:, :, :, 1, :], mul=-1.0), reads=[sk], writes=[wk])
                        E.op("pool", lambda e: e.tensor_copy(out=qro[:, :, :, 1, :], in_=qvh[:, :, :, 0, :]), reads=[sk], writes=[wk])
                        E.op("pool", lambda e: e.tensor_copy(out=wb_[:, C_K:C_K + 128], in_=s_[:, 512:640]), reads=[sk], writes=[wk])
                        kvh = s_[:, 512:640].rearrange("p (a t d) -> p a t d", a=2, t=2)
                        kro = wb_[:, C_KR:C_KR + 128].rearrange("p (a t d) -> p a t d", a=2, t=2)
                        E.op("act", lambda e: e.mul(out=kro[:, :, 0, :], in_=kvh[:, :, 1, :], mul=-1.0), reads=[sk], writes=[wk])
                        E.op("pool", lambda e: e.tensor_copy(out=kro[:, :, 1, :], in_=kvh[:, :, 0, :]), reads=[sk], writes=[wk])
                        E.op("act", lambda e: e.copy(out=wb_[:, C_SU:C_SU + 256], in_=s_[:, 768:1024]), reads=[sk], writes=[wk])
                        E.op("act", lambda e: e.copy(out=wb_[:, C_VSV:C_VSV + 128], in_=s_[:, 640:768]), reads=[sk], writes=[wk])
                        E.op("pool", lambda e: e.tensor_copy(out=wb_[:, C_VSV + 128:C_VSV + 256], in_=s_[:, 1024:1152]), reads=[sk], writes=[wk])
                    for hd in range(8):
                        sk = ("stg", 0)
                        sv_ = stg[0:64, 0, 0:1024]
                        E.dma("sp", sv_, w_out[l, hd * 64:(hd + 1) * 64, :], writes=[sk])
                        E.op("dve", lambda e: e.tensor_copy(out=woA[:, hd, :], in_=sv_), reads=[sk], writes=["woA"])
                    for c in range(6):
                        sk = ("stg", 0)
                        s_ = stg[:, 0, 0:1024]
                        E.dma("sp", s_, w_out[l, 512 + c * 128:512 + (c + 1) * 128, :], writes=[sk])
                        E.op("pool" if c % 2 else "dve", lambda e: e.tensor_copy(out=woB[:, c, :], in_=s_), reads=[sk], writes=["woB"])
                    for h in range(4):
                        sk = ("stg", 0)
                        s_ = stg[:, 0, 0:128]
                        E.dma("sp", s_, sgu_ws[l, h, :, :], writes=[sk])
                        E.op("pe", lambda e: e.transpose(out=pA[:, 0:128], in_=s_, identity=ident_f[:]), reads=[sk, "ident_f"], writes=["pA"])
                        E.op("act", lambda e: e.copy(out=wsT[:, h, :], in_=pA[:, 0:128]), reads=["pA"], writes=["wsT"])
                        E.dma("sp", bsT[(h % 2) * 64:(h % 2) * 64 + 64, h // 2, :], sgu_b[l, h, :].partition_broadcast(64), writes=["bsT"])
                    bcast_load(ph, lng[:], sgu_ln_g[l, :], "lng"); bcast_load(ph, lnb[:], sgu_ln_b[l, :], "lnb")
                    E.op("pool", lambda e: e.memset(PWf[:], 0.0), writes=["PWf"])
                    for g4 in range(4):
                        E.dma("sp", PWf[(g4 % 2) * 64:(g4 % 2) * 64 + 64, g4 // 2, (g4 % 2) * 64:(g4 % 2) * 64 + 64], pool_w[l, g4, :, :], reads=[], writes=["PWf"])
                    E.op("pool", lambda e: e.tensor_copy(out=PWm[:], in_=PWf[:]), reads=["PWf"], writes=["PW"])
                    for c in range(2):
                        E.dma("sp", pscale[:, c:c + 1], pool_scale[l, c * 128:(c + 1) * 128].rearrange("(p o) -> p o", o=1), writes=["pscale"])
                        for j in range(3):
                            E.dma("sp", cw[:, c, j:j + 1], conv_w[l, j, c * 128:(c + 1) * 128].rearrange("(p o) -> p o", o=1), writes=["cw"])
                    bcast_load(ph, es8[:], attn_sink[l, :], "es8")
                    E.op("act", lambda e: e.activation(out=es8[:], in_=es8[:], func=AF.Exp), reads=["es8"], writes=["es8"])
                    E.op("pool", lambda e: e.tensor_copy(out=esink[:], in_=es8[0:64, :].unsqueeze(2).to_broadcast([64, 8, 128])), reads=["es8"], writes=["esink"])

                    chk(2)
                    compute_mod(sg, l, 0, 1, modT, stage_ap, "tmpf", csb_ap, "xt")
                    finish_mod(sg, l, norm1_g, modT, gts)
                E.barrier()
                with ExitStack() as tp:
                    smallst = (ssq, rt)
                    E.op("pool", lambda e: e.memset(vnz[:], 0.0), writes=["vnz"])
                    rot = {"i": 0}

                    def proj_group(tiles, src, src_row0, slot, par, cosT, sinT, ckey, kdst, kkey, vdst, vkey, xpdst, xpkey, ydst, ykey, flag_tiles, hooks=()):
                        hooks = list(hooks)
                        for (ts, rtile, ti) in tiles:
                            if hooks:
                                hooks.pop(0)()
                            E.dma("act", xt[:], src[rtile * 128:(rtile + 1) * 128, :], writes=["xt"])
                            norm_mod(xt[:], "xt", modT, hb[:], "hb", smallst, tmpf, "tmpf")
                            for kc in range(8):
                                E.op("pe", lambda e: e.transpose(out=pT[:, kc, :], in_=hb[:, kc * 128:(kc + 1) * 128], identity=ident_b[:]),
                                     reads=["hb", "ident_b"], writes=["pT"])
                            E.op("act", lambda e: e.copy(out=hT[:, :, ts * 128:(ts + 1) * 128], in_=pT[:]), reads=["pT"], writes=[("hT", ts)])
                            for kc in range(8):
                                E.op("pe", lambda e: e.matmul(pC[:, 0:384], lhsT=hT[:, kc, ts * 128:(ts + 1) * 128], rhs=winb[:, kc, C_VSV:C_VSV + 384],
                                                              start=(kc == 0), stop=(kc == 7)), reads=[("hT", ts), ("winb", kc)], writes=["pC"])
                            E.op("dve", lambda e: e.tensor_copy(out=vdst[:, ts, :], in_=pC[:, 0:128]), reads=["pC"], writes=[vkey])
                            E.op("pool", lambda e: e.memset(s1[:], 0.0), writes=["s1"])
                            E.op("pool", lambda e: e.memset(s2[:], 0.0), writes=["s2"])
                            E.op("act", lambda e: e.activation(out=svg[:], in_=pC[:, 128:384], func=AF.Gelu, accum_out=s1[:]), reads=["pC"], writes=["svg", "s1"])
                            E.op("dve", lambda e: e.tensor_scalar(out=s1[:], in0=s1[:], scalar1=-1.0 / 256, scalar2=None, op0=ALU.mult), reads=["s1"], writes=["s1"])
                            E.op("act", lambda e: e.activation(out=vn[:], in_=svg[:], func=AF.Square, bias=s1[:, 0:1], accum_out=s2[:]), reads=["svg", "s1"], writes=["vn", "s2"])
                            E.op("act", lambda e: e.activation(out=s3[:], in_=s2[:], func=AF.Sqrt, scale=1.0 / 256, bias=EPS), reads=["s2"], writes=["s3"])
                            E.op("dve", lambda e: e.reciprocal(out=s3[:], in_=s3[:]), reads=["s3"], writes=["s3"])
                            E.op("dve", lambda e: e.tensor_scalar(out=vn[:], in0=svg[:], scalar1=s1[:, 0:1], scalar2=s3[:, 0:1], op0=ALU.add, op1=ALU.mult),
                                 reads=["svg", "s1", "s3"], writes=["vn"])
                            E.op("pool", lambda e: e.tensor_tensor(out=vn[:], in0=vn[:], in1=lng[:], op=ALU.mult), reads=["vn", "lng"], writes=["vn"])
                            vn4 = vn[:].rearrange("p (h d) -> p h d", h=4)
                            lb4 = lnb[:].rearrange("p (h d) -> p h d", h=4)
                            for par2 in range(2):
                                E.op("pool", lambda e: e.tensor_tensor(out=vnz[:, par2::2, par2 * 64:par2 * 64 + 64], in0=vn4[:, par2::2, :], in1=lb4[:, par2::2, :], op=ALU.add),
                                     reads=["vn", "lnb"], writes=["vnz"])
                            for c in range(2):
                                E.op("pe", lambda e: e.matmul(pB[:, c * 128:(c + 1) * 128], lhsT=vnz[:, 2 * c, :], rhs=wsT[:, 2 * c, :], start=True, stop=False),
                                     reads=["vnz", "wsT"], writes=["pB"])
                                E.op("pe", lambda e: e.matmul(pB[:, c * 128:(c + 1) * 128], lhsT=vnz[:, 2 * c + 1, :], rhs=wsT[:, 2 * c + 1, :], start=False, stop=True),
                                     reads=["vnz", "wsT"], writes=["pB"])
                            E.op("dve", lambda e: e.tensor_tensor(out=t1[:, 0:256].rearrange("p (c q) -> p c q", c=2), in0=pB[:, 0:256].rearrange("p (c q) -> p c q", c=2), in1=bsT[:], op=ALU.add),
                                 reads=["pB", "bsT"], writes=["t1"])
                            E.op("pool", lambda e: e.tensor_copy(out=ybT[:, par, :, ts * 128:(ts + 1) * 128], in_=t1[:, 0:256].rearrange("p (c q) -> p c q", c=2)),
                                 reads=["t1"], writes=[("ybT", par)])
                        while hooks:
                            hooks.pop(0)()
                        tsl = [t[0] for t in tiles]
                        c0, c1 = min(tsl) * 128, (max(tsl) + 1) * 128
                        n = c1 - c0
                        hkeys = [("hT", t) for t in tsl]

                        def fm(col, pp):
                            for kc in range(8):
                                E.op("pe", lambda e: e.matmul(pp[:, 0:n], lhsT=winb[:, kc, col:col + 128], rhs=hT[:, kc, c0:c1], start=(kc == 0), stop=(kc == 7)),
                                     reads=hkeys + [("winb", kc)], writes=[PN[id(pp)]])
                        for j in range(5):
                            ca, cb_ = (C_Q + j * 128, C_QR + j * 128) if j < 4 else (C_K, C_KR)
                            fm(ca, pA); fm(cb_, pB)
                            E.op("dve", lambda e: e.tensor_tensor(out=t1[:, 0:n], in0=pA[:, 0:n], in1=cosT[:, c0:c1], op=ALU.mult), reads=["pA", ckey], writes=["t1"])
                            E.op("dve", lambda e: e.tensor_tensor(out=t2[:, 0:n], in0=pB[:, 0:n], in1=sinT[:, c0:c1], op=ALU.mult), reads=["pB", ckey], writes=["t2"])
                            if j < 4:
                                E.op("pool", lambda e: e.tensor_tensor(out=qT[:, par, j, c0:c1], in0=t1[:, 0:n], in1=t2[:, 0:n], op=ALU.add), reads=["t1", "t2"], writes=[("qT", par)])
                            else:
                                E.op("pool", lambda e: e.tensor_tensor(out=kdst[:, c0:c1], in0=t1[:, 0:n], in1=t2[:, 0:n], op=ALU.add), reads=["t1", "t2"], writes=[kkey])
                        for c in range(2):
                            fm(C_SU + c * 128, pA)
                            E.op("act", lambda e: e.activation(out=suT[:, par, c, c0:c1], in_=pA[:, 0:n], func=AF.Gelu), reads=["pA"], writes=[("suT", par)])
                            fm(C_PO + c * 128, pB)
                            E.op("act", lambda e: e.copy(out=xpdst[:, c, c0:c1], in_=pB[:, 0:n]), reads=["pB"], writes=[xpkey])
                            fm(C_CB + c * 128, pA)
                            E.op("act", lambda e: e.copy(out=cbT[:, par, c, c0:c1], in_=pA[:, 0:n]), reads=["pA"], writes=[("cbT", par)])
                            fm(C_CC + c * 128, pA); fm(C_CX + c * 128, pB)
                            E.op("act", lambda e: e.copy(out=t1[:, 0:n], in_=pA[:, 0:n]), reads=["pA"], writes=["t1"])
                            E.op("dve", lambda e: e.tensor_tensor(out=ydst[:, c, c0:c1], in0=t1[:, 0:n], in1=pB[:, 0:n], op=ALU.mult), reads=["t1", "pB"], writes=[ykey])
                        E.op("pool", lambda e: e.tensor_tensor(out=ybT[:, par, :, c0:c1], in0=ybT[:, par, :, c0:c1], in1=suT[:, par, :, c0:c1], op=ALU.mult),
                             reads=[("ybT", par), ("suT", par)], writes=[("ybT", par)])
                        for (ts, fcol) in flag_tiles:
                            E.op("pool", lambda e: e.tensor_scalar(out=xpdst[:, :, ts * 128:(ts + 1) * 128], in0=xpdst[:, :, ts * 128:(ts + 1) * 128], scalar1=flg[:, fcol:fcol + 1], scalar2=None, op0=ALU.mult),
                                 reads=[xpkey, "flg"], writes=[xpkey])
                            E.op("pool", lambda e: e.tensor_scalar(out=ydst[:, :, ts * 128:(ts + 1) * 128], in0=ydst[:, :, ts * 128:(ts + 1) * 128], scalar1=flg[:, fcol:fcol + 1], scalar2=None, op0=ALU.mult),
                                 reads=[ykey, "flg"], writes=[ykey])

                    def mix_tile(ts, par, keyblocks, winX, winY, tabidx, src, rtile, gmod, dst, drow):
                        qc0 = ts * 128
                        E.dma("act", xr[:], src[rtile * 128:(rtile + 1) * 128, :], writes=["xr"])
                        for gg in range(2):
                            pr = slice(gg * 64, gg * 64 + 64)
                            for bi, (kv_, kk_, vv_, vk_, mi) in enumerate(keyblocks):
                                pS = pA if bi % 2 == 0 else pB
                                E.op("pe", lambda e: e.matmul(pS[:].rearrange("p (j q) -> p j q", j=4), lhsT=kv_[pr, :], rhs=qT[pr, par, :, qc0:qc0 + 128], start=True, stop=True),
                                     reads=[kk_, ("qT", par)], writes=[PN[id(pS)]])
                                pe_ = pexp[:, bi % 2, :]
                                pk = ("pexp", bi % 2)
                                E.op("act", lambda e: e.activation(out=pe_, in_=pS[:], func=AF.Exp, scale=0.125), reads=[PN[id(pS)]], writes=[pk])
                                if mi is not None:
                                    E.op("pool", lambda e: e.tensor_tensor(out=pe_.rearrange("p (j q) -> p j q", j=4), in0=pe_.rearrange("p (j q) -> p j q", j=4),
                                                                            in1=mk[:, mi, :].unsqueeze(1).to_broadcast([128, 4, 128]), op=ALU.mult), reads=[pk, "mk"], writes=[pk])
                                first, lastb = (bi == 0), (bi == len(keyblocks) - 1)
                                E.op("pe", lambda e: e.matmul(pO[0:64, :], lhsT=vv_[:, pr], rhs=pe_, start=first, stop=lastb), reads=[vk_, pk], writes=["pO"])
                                E.op("pe", lambda e: e.matmul(pDen[0:64, :], lhsT=ones_b[:, 0:64], rhs=pe_, start=first, stop=lastb), reads=["ones_b", pk], writes=["pDen"])
                            E.op("dve", lambda e: e.tensor_tensor(out=den[:].rearrange("p (j q) -> p j q", j=4), in0=pDen[0:64, :].rearrange("p (j q) -> p j q", j=4), in1=esink[:, gg * 4:gg * 4 + 4, :], op=ALU.add),
                                 reads=["pDen", "esink"], writes=["den"])
                            E.op("dve", lambda e: e.reciprocal(out=den[:], in_=den[:]), reads=["den"], writes=["den"])
                            E.op("dve", lambda e: e.tensor_tensor(out=attnT[:, gg, :, :], in0=pO[0:64, :].rearrange("p (j q) -> p j q", j=4), in1=den[:].rearrange("p (j q) -> p j q", j=4), op=ALU.mult),
                                 reads=["pO", "den"], writes=["attnT"])
                        for (d0, ap_, k_) in winX:
                            nn = ap_.shape[-1]
                            E.op("pool", lambda e: e.tensor_copy(out=Xw[:, :, d0:d0 + nn], in_=ap_), reads=[k_], writes=["Xw"])
                        E.op("pool", lambda e: e.tensor_tensor(out=A2[:, :, 1:160], in0=Xw[:, :, 1:160], in1=Xw[:, :, 0:159], op=ALU.add), reads=["Xw"], writes=["A2"])
                        E.op("pool", lambda e: e.tensor_tensor(out=A4[:, :, 2:159], in0=A2[:, :, 3:160], in1=A2[:, :, 1:158], op=ALU.add), reads=["A2"], writes=["A4"])
                        E.op("pool", lambda e: e.tensor_tensor(out=A8[:, :, 4:157], in0=A4[:, :, 6:159], in1=A4[:, :, 2:155], op=ALU.add), reads=["A4"], writes=["A8"])
                        E.op("pool", lambda e: e.tensor_tensor(out=A16[:, :, 8:153], in0=A8[:, :, 12:157], in1=A8[:, :, 4:149], op=ALU.add), reads=["A8"], writes=["A16"])
                        for (c, half, lev, lk) in ((0, 0, A2, "A2"), (0, 1, A4, "A4"), (1, 0, A8, "A8"), (1, 1, A16, "A16")):
                            pr = slice(half * 64, half * 64 + 64)
                            E.op("dve", lambda e: e.tensor_tensor(out=pd[pr, c, :], in0=lev[pr, c, 16:144], in1=ptb[pr, tabidx, c, :], op=ALU.mult), reads=[lk, "ptb"], writes=["pd"])
                        E.op("pool", lambda e: e.tensor_tensor(out=dT[:], in0=pd[:], in1=Xw[:, :, 16:144], op=ALU.subtract), reads=["pd", "Xw"], writes=["dT"])
                        for c in range(2):
                            E.op("pe", lambda e: e.matmul(pC[:, c * 128:(c + 1) * 128], lhsT=PWm[:, c, :], rhs=dT[:, c, :], start=True, stop=True), reads=["PW", "dT"], writes=["pC"])
                            E.op("dve", lambda e: e.tensor_scalar(out=ycT[:, c, :], in0=pC[:, c * 128:(c + 1) * 128], scalar1=pscale[:, c:c + 1], scalar2=None, op0=ALU.mult),
                                 reads=["pC", "pscale"], writes=["ycT"])
                        for (d0, ap_, k_) in winY:
                            nn = ap_.shape[-1]
                            E.op("pool", lambda e: e.tensor_copy(out=Yw[:, :, d0:d0 + nn], in_=ap_), reads=[k_], writes=["Yw"])
                        for c in range(2):
                            E.op("dve", lambda e: e.tensor_scalar(out=zc[:, c, :], in0=Yw[:, c, 0:128], scalar1=cw[:, c, 0:1], scalar2=None, op0=ALU.mult), reads=["Yw", "cw"], writes=["zc"])
                            E.op("dve", lambda e: e.scalar_tensor_tensor(out=zc[:, c, :], in0=Yw[:, c, 1:129], scalar=cw[:, c, 1:2], in1=zc[:, c, :], op0=ALU.mult, op1=ALU.add), reads=["Yw", "cw", "zc"], writes=["zc"])
                            E.op("dve", lambda e: e.scalar_tensor_tensor(out=zc[:, c, :], in0=Yw[:, c, 2:130], scalar=cw[:, c, 2:3], in1=zc[:, c, :], op0=ALU.mult, op1=ALU.add), reads=["Yw", "cw", "zc"], writes=["zc"])
                        E.op("pool", lambda e: e.tensor_tensor(out=ydT[:], in0=zc[:], in1=cbT[:, par, :, qc0:qc0 + 128], op=ALU.mult), reads=["zc", ("cbT", par)], writes=["ydT"])
                        for nh in range(2):
                            ns = slice(nh * 512, nh * 512 + 512)
                            for hd in range(8):
                                E.op("pe", lambda e: e.matmul(pW[nh][:], lhsT=attnT[:, hd // 4, hd % 4, :], rhs=woA[:, hd, ns], start=(hd == 0), stop=False), reads=["attnT", "woA"], writes=[("pW", nh)])
                            srcs = [(ybT[:, par, 0, qc0:qc0 + 128], ("ybT", par)), (ybT[:, par, 1, qc0:qc0 + 128], ("ybT", par)),
                                    (ycT[:, 0, :], "ycT"), (ycT[:, 1, :], "ycT"), (ydT[:, 0, :], "ydT"), (ydT[:, 1, :], "ydT")]
                            for c, (ap_, k_) in enumerate(srcs):
                                E.op("pe", lambda e: e.matmul(pW[nh][:], lhsT=ap_, rhs=woB[:, c, ns], start=False, stop=(c == 5)), reads=[k_, "woB"], writes=[("pW", nh)])
                        for nh in range(2):
                            ns = slice(nh * 512, nh * 512 + 512)
                            E.op("dve", lambda e: e.tensor_tensor(out=tmpM[:, ns], in0=pW[nh][:], in1=gmod[:, 2, ns], op=ALU.mult), reads=[("pW", nh), "modT"], writes=["tmpM"])
                        E.op("pool", lambda e: e.tensor_tensor(out=tmpM[:], in0=tmpM[:], in1=xr[:], op=ALU.add), reads=["tmpM", "xr"], writes=["tmpM"])
                        E.dma("sp", dst[drow * 128:(drow + 1) * 128, :], tmpM[:], reads=["tmpM"], writes=[("dram", id(dst), drow)])

                    xpC = SB(tp, nm("xpC"), [128, 2, 256], BF16); yC = SB(tp, nm("yC"), [128, 2, 256], BF16)
                    zpad = SB(tp, nm("zpad"), [128, 2, 16], BF16)
                    E.op("pool", lambda e: e.memset(zpad[:], 0.0), writes=["zpad"])
                    chk(3)
                    ctiles = [(0, 0, 0), (1, 1, 1)]
                    proj_group(ctiles, csrc, 0, None, 1, cs_t[:, 1, :], sn_t[:, 1, :], ("cs", 1), kcT, "kcT", Vc, "Vc", xpC, "xpC", yC, "yC", [])
                    chk(4)
                    ctxblocks = [(kcT[:, 0:128], "kcT", Vc[:, 0, :], "Vc", None), (kcT[:, 128:256], "kcT", Vc[:, 1, :], "Vc", None)]
                    if not last:
                        for ts in range(2):
                            wx = [(0, zpad[:, :, 0:16], "zpad") if ts == 0 else (0, xpC[:, :, 112:128], "xpC"),
                                  (16, xpC[:, :, ts * 128:(ts + 1) * 128], "xpC"),
                                  (144, xpC[:, :, 128:144], "xpC") if ts == 0 else (144, zpad[:, :, 0:16], "zpad")]
                            wy = [(0, zpad[:, :, 0:1], "zpad") if ts == 0 else (0, yC[:, :, 127:128], "yC"),
                                  (1, yC[:, :, ts * 128:(ts + 1) * 128], "yC"),
                                  (129, yC[:, :, 128:129], "yC") if ts == 0 else (129, zpad[:, :, 0:1], "zpad")]
                            mix_tile(ts, 1, ctxblocks, wx, wy, 3 + ts, csrc, ts, modT, xcm_s, ts)
                    chk(5)
                    compute_mod(None, l, 0, 0, modT, stage_ap, "tmpf", csb_ap, "xt")
                    finish_mod(None, l, norm1_g, modT, gts)
                    E.barrier()

                    ngroups = NT // 4

                    def P(g, hooks=()):
                        tiles = [(ts, g * 4 + ts, g * 4 + ts) for ts in range(4) if p_lo <= g * 4 + ts < p_hi]
                        slot = g % 3
                        par = g % 2
                        E.dma("act", cs_t[:, par, :], rope[0, :, g * 512:(g + 1) * 512], writes=[("cs", par)])
                        E.dma("act", sn_t[:, par, :], rope[1, :, g * 512:(g + 1) * 512], writes=[("cs", par)])
                        fl = []
                        if g == 0:
                            fl.append((1, 1))
                        if g == ngroups - 1:
                            fl.append((2, 2))
                        proj_group(tiles, xsrc, 0, slot, par, cs_t[:, par, :], sn_t[:, par, :], ("cs", par),
                                   kT[:, slot, :], ("kT", slot), Vt[:, slot, :, :], ("V", slot),
                                   xpT[:, slot, :, :], ("xpT", slot), yT[:, slot, :, :], ("yT", slot), fl, hooks)

                    def M(g, tss):
                        par = g % 2
                        for ts in tss:
                            ti = g * 4 + ts

                            def tl(t):
                                return (t // 4) % 3, t % 4
                            blocks = []
                            for t, mi in ((ti - 1, 2 if ti == 2 else 0), (ti, None), (ti + 1, 3 if ti == NT - 3 else 1)):
                                sl, tt = tl(t)
                                blocks.append((kT[:, sl, tt * 128:(tt + 1) * 128], ("kT", sl), Vt[:, sl, tt, :], ("V", sl), mi))
                            blocks += ctxblocks
                            (sp_, tp_), (sc_, tc_), (sn_, tn_) = tl(ti - 1), tl(ti), tl(ti + 1)
                            wx = [(0, xpT[:, sp_, :, tp_ * 128 + 112:tp_ * 128 + 128], ("xpT", sp_)),
                                  (16, xpT[:, sc_, :, tc_ * 128:(tc_ + 1) * 128], ("xpT", sc_)),
                                  (144, xpT[:, sn_, :, tn_ * 128:tn_ * 128 + 16], ("xpT", sn_))]
                            wy = [(0, yT[:, sp_, :, tp_ * 128 + 127:tp_ * 128 + 128], ("yT", sp_)),
                                  (1, yT[:, sc_, :, tc_ * 128:(tc_ + 1) * 128], ("yT", sc_)),
                                  (129, yT[:, sn_, :, tn_ * 128:tn_ * 128 + 1], ("yT", sn_))]
                            tabidx = 1 if ti == 2 else (2 if ti == NT - 3 else 0)
                            mix_tile(ts, par, blocks, wx, wy, tabidx, xsrc, ti, modT, xm_s, ti)

                    chk(6)
                    P(0)
                    for g in range(ngroups):
                        mt = [ts for ts in range(4) if m_lo <= g * 4 + ts < m_hi]
                        if g + 1 < ngroups:
                            early = [ts for ts in mt if ts < 3]
                            late = [ts for ts in mt if ts == 3]
                            P(g + 1, [(lambda ts=ts, g=g: M(g, [ts])) for ts in early])
                            M(g, late)
                        else:
                            M(g, mt)
            chk(7)
            E.barrier()
            with ExitStack() as ph:
                modT = SB(ph, nm("modT2"), [128, 1 if last else 2, 3, D])
                chk(71)
                fg = SB(ph, nm("fg"), [128, D if last else 1])
                if last:
                    bcast_load(ph, fg[:], final_g, "fg")
                rw = SB(ph, nm("rw"), [128, 8, 32]); rb = SB(ph, nm("rb"), [1, 32])
                E.dma("sp", rw[:], router_w[l].rearrange("(c p) n -> p c n", p=128), writes=["rw"])
                E.dma("sp", rb[:], router_b[l:l + 1, :], writes=["rb"])
                TB = 7 if last else 6
                w1b = SB(ph, nm("w1b"), [128, 2, 8, 2048], BF16); w2b = SB(ph, nm("w2b"), [128, 2, 8, D], BF16)
                b1t = SB(ph, nm("b1t"), [128, 2, 8, 2]); GTb = SB(ph, nm("GTb"), [32, 128], BF16); B2b = SB(ph, nm("B2b"), [32, D], BF16)
                E.dma("pool", B2b[:], exp_b2[l, :, :], writes=["B2b"])
                h2T = SB(ph, nm("h2T"), [128, TB, 8, 128], BF16); acc = SB(ph, nm("acc"), [128, TB, D])
                Gt = SB(ph, nm("G"), [128, TB, 32])
                xt = SB(ph, nm("xt2"), [128, D]); tmpf = SB(ph, nm("tmpf2"), [128, D]); h2 = SB(ph, nm("h2"), [128, D])
                h2T32 = tmpf[:].rearrange("p (c n) -> p c n", c=8)
                lg = SB(ph, nm("lg"), [128, 32]); mx8 = SB(ph, nm("mx8"), [128, 8]); msk = SB(ph, nm("msk"), [128, 32])
                ex = SB(ph, nm("ex"), [128, 32]); sm = SB(ph, nm("sm"), [128, 1]); ngm = SB(ph, nm("ngm"), [128, 1])
                ssq = SB(ph, nm("ssq2"), [128, 1]); rt = SB(ph, nm("rt2"), [128, 1])
                gc = SB(ph, nm("gc"), [128, 2, 512]); sgm = h2[:].rearrange("p (s n) -> p s n", s=2); lc = SB(ph, nm("lc"), [128, 2, 512])
                aT = SB(ph, nm("aT"), [128, 2, 8, 512], BF16)
                pZ = [pA, pB, pC, pO]
                chk(72)
                for which in ((0, 1) if not last else (0,)):
                    compute_mod(None, l, 1, which, modT[:, which, :, :], tmpf[:].rearrange("p (c n) -> p c n", c=8), "tmpf", xt[:].rearrange("p (c n) -> p c n", c=8), "xt")
                    finish_mod(None, l, norm2_g, modT[:, which, :, :], [(h2[:, 0:512], "h2"), (h2[:, 512:1024], "h2")])
                E.barrier()
                pTf = pDen

                chk(725)
                if not last:
                    work = [(xm_s, ti, 0, x1_s, ti) for ti in range(1, NT - 1)] + [(xcm_s, t, 1, xc1_s, t) for t in range(2)]
                else:
                    work = [(xm_s, ti, 0, yout, ti - 2) for ti in range(2, NT - 2)]
                bsizes = [7, 7, 6, 6, 6] if last else [6] * 6
                assert sum(bsizes) == len(work)
                bstarts = [sum(bsizes[:i]) for i in range(len(bsizes))]
                for b0, bsz in list(zip(bstarts, bsizes))[:nblk]:
                    blk = work[b0:b0 + bsz]
                    for bi, (srcT, rtile, which, dstT, drow) in enumerate(blk):
                        mT = modT[:, which, :, :]
                        E.dma("sp", xt[:], srcT[rtile * 128:(rtile + 1) * 128, :], writes=["xt"])
                        chk(730)
                        norm_mod(xt[:], "xt", mT, h2[:], "h2", (ssq, rt), tmpf, "tmpf")
                        chk(731)
                        for half in range(2):
                            for j in range(4):
                                kc = half * 4 + j
                                E.op("pe", lambda e: e.transpose(out=pTf[:, j * 128:(j + 1) * 128], in_=h2[:, kc * 128:(kc + 1) * 128], identity=ident_f[:]),
                                     reads=["h2", "ident_f"], writes=["pDen"])
                            chk(732)
                            E.op("act", lambda e: e.copy(out=h2T32[:, half * 4:half * 4 + 4, :], in_=pTf[:].rearrange("p (j q) -> p j q", j=4)), reads=["pDen"], writes=["tmpf"])
                            chk(733)
                            E.op("act", lambda e: e.copy(out=h2T[:, bi, half * 4:half * 4 + 4, :], in_=pTf[:].rearrange("p (j q) -> p j q", j=4)), reads=["pDen"], writes=[("h2T", bi)])
                        chk(73)
                        for kc in range(8):
                            E.op("pe", lambda e: e.matmul(pW[0][:, 0:32], lhsT=h2T32[:, kc, :], rhs=rw[:, kc, :], start=(kc == 0), stop=False), reads=["tmpf", "rw"], writes=[("pW", 0)])
                        E.op("pe", lambda e: e.matmul(pW[0][:, 0:32], lhsT=ones_f[0:1, :], rhs=rb[0:1, :], start=False, stop=True), reads=["ones_f", "rb"], writes=[("pW", 0)])
                        chk(74)
                        E.op("dve", lambda e: e.tensor_copy(out=lg[:], in_=pW[0][:, 0:32]), reads=[("pW", 0)], writes=["lg"])
                        E.op("dve", lambda e: e.max(out=mx8[:], in_=lg[:]), reads=["lg"], writes=["mx8"])
                        E.op("dve", lambda e: e.tensor_scalar(out=msk[:], in0=lg[:], scalar1=mx8[:, 3:4], scalar2=None, op0=ALU.is_ge), reads=["lg", "mx8"], writes=["msk"])
                        chk(75)
                        E.op("dve", lambda e: e.tensor_scalar(out=ngm[:], in0=mx8[:, 0:1], scalar1=-1.0, scalar2=None, op0=ALU.mult), reads=["mx8"], writes=["ngm"])
                        E.op("act", lambda e: e.activation(out=ex[:], in_=lg[:], func=AF.Exp, bias=ngm[:, 0:1]), reads=["lg", "ngm"], writes=["ex"])
                        E.op("dve", lambda e: e.tensor_tensor(out=ex[:], in0=ex[:], in1=msk[:], op=ALU.mult), reads=["ex", "msk"], writes=["ex"])
                        E.op("dve", lambda e: e.reduce_sum(out=sm[:], in_=ex[:], axis=AX.X), reads=["ex"], writes=["sm"])
                        E.op("dve", lambda e: e.reciprocal(out=sm[:], in_=sm[:]), reads=["sm"], writes=["sm"])
                        E.op("dve", lambda e: e.tensor_scalar(out=Gt[:, bi, :], in0=ex[:], scalar1=sm[:, 0:1], scalar2=None, op0=ALU.mult), reads=["ex", "sm"], writes=[("G", bi)])
                        E.op("pe", lambda e: e.transpose(out=pTf[0:32, 0:128], in_=Gt[:, bi, :], identity=ident_f[:]), reads=[("G", bi), "ident_f"], writes=["pDen"])
                        E.op("act", lambda e: e.copy(out=GTb[:], in_=pTf[0:32, 0:128]), reads=["pDen"], writes=["GTb"])
                        for nh in range(2):
                            E.op("pe", lambda e: e.matmul(pW[nh][:], lhsT=GTb[:], rhs=B2b[:, nh * 512:(nh + 1) * 512], start=True, stop=True), reads=["GTb", "B2b"], writes=[("pW", nh)])
                            E.op("act", lambda e: e.copy(out=acc[:, bi, nh * 512:(nh + 1) * 512], in_=pW[nh][:]), reads=[("pW", nh)], writes=[("acc", bi)])
                    chk(8)
                    nb_t = len(blk)

                    def load_w(ex_i, wpar):
                        for kc in range(8):
                            E.dma("pool", w1b[:, wpar, kc, :], exp_w1[l, ex_i, kc * 128:(kc + 1) * 128, :], writes=[("w1b", wpar, kc)])
                        E.dma("sp", b1t[:, wpar, :, :], exp_b1[l, ex_i, :].rearrange("(c p t) -> p c t", p=128, t=2), writes=[("b1t", wpar)])
                        E.op("dve", lambda e: e.tensor_scalar(out=b1t[:, wpar, :, 1], in0=b1t[:, wpar, :, 1], scalar1=1.0, scalar2=None, op0=ALU.add), reads=[("b1t", wpar)], writes=[("b1t", wpar)])
                        for kc in range(0, 8, 2):
                            E.dma("pool", w2b[:, wpar, kc:kc + 2, :], exp_w2[l, ex_i, kc * 128:(kc + 2) * 128, :].rearrange("(c p) n -> p c n", p=128),
                                  writes=[("w2b", wpar, kc), ("w2b", wpar, kc + 1)])

                    groups = [(t0, min(4, nb_t - t0)) for t0 in range(0, nb_t, 4)]
                    items = [(ex_i, gi) for ex_i in range(nexp) for gi in range(len(groups))]

                    def U(it_i):
                        ex_i, gi = items[it_i]
                        wpar = ex_i % 2
                        ap = it_i % 2
                        t0, nt = groups[gi]
                        n = nt * 128
                        hk = [("h2T", t0 + t) for t in range(nt)]
                        for fc in range(8):
                            pg, pl_ = (pA, pB) if fc % 2 == 0 else (pC, pO)
                            s_ = fc % 2
                            for (pz, off) in ((pg, 0), (pl_, 1)):
                                for kc in range(8):
                                    E.op("pe", lambda e: e.matmul(pz[:, 0:n].rearrange("p (t q) -> p t q", t=nt), lhsT=w1b[:, wpar, kc, fc * 256 + off:fc * 256 + 256:2], rhs=h2T[:, t0:t0 + nt, kc, :],
                                                                  start=(kc == 0), stop=(kc == 7)), reads=hk + [("w1b", wpar, kc)], writes=[PN[id(pz)]])
                            E.op("dve", lambda e: e.tensor_scalar(out=gc[:, s_, 0:n], in0=pg[:, 0:n], scalar1=b1t[:, wpar, fc, 0:1], scalar2=7.0, op0=ALU.add, op1=ALU.min),
                                 reads=[PN[id(pg)], ("b1t", wpar)], writes=[("gc", s_)])
                            E.op("act", lambda e: e.activation(out=sgm[:, s_, 0:n], in_=gc[:, s_, 0:n], func=AF.Sigmoid, scale=1.702), reads=[("gc", s_)], writes=[("sgm", s_), "h2"])
                            E.op("dve", lambda e: e.tensor_scalar(out=lc[:, s_, 0:n], in0=pl_[:, 0:n], scalar1=b1t[:, wpar, fc, 1:2], scalar2=-6.0, op0=ALU.add, op1=ALU.max),
                                 reads=[PN[id(pl_)], ("b1t", wpar)], writes=[("lc", s_)])
                            E.op("dve", lambda e: e.tensor_tensor(out=gc[:, s_, 0:n], in0=gc[:, s_, 0:n], in1=sgm[:, s_, 0:n], op=ALU.mult), reads=[("gc", s_), ("sgm", s_), "h2"], writes=[("gc", s_)])
                            E.op("dve", lambda e: e.scalar_tensor_tensor(out=aT[:, ap, fc, 0:n], in0=lc[:, s_, 0:n], scalar=8.0, in1=gc[:, s_, 0:n], op0=ALU.min, op1=ALU.mult),
                                 reads=[("lc", s_), ("gc", s_)], writes=[("aT", ap)])

                    def Dn(it_i):
                        ex_i, gi = items[it_i]
                        wpar = ex_i % 2
                        ap = it_i % 2
                        t0, nt = groups[gi]
                        for t in range(nt):
                            bi = t0 + t
                            for nh in range(2):
                                for kc in range(8):
                                    E.op("pe", lambda e: e.matmul(pW[nh][:], lhsT=aT[:, ap, kc, t * 128:(t + 1) * 128], rhs=w2b[:, wpar, kc, nh * 512:(nh + 1) * 512], start=(kc == 0), stop=(kc == 7)),
                                         reads=[("aT", ap), ("w2b", wpar, kc)], writes=[("pW", nh)])
                                E.op("dve", lambda e: e.scalar_tensor_tensor(out=acc[:, bi, nh * 512:(nh + 1) * 512], in0=pW[nh][:], scalar=Gt[:, bi, ex_i:ex_i + 1],
                                                                              in1=acc[:, bi, nh * 512:(nh + 1) * 512], op0=ALU.mult, op1=ALU.add),
                                     reads=[("pW", nh), ("G", bi), ("acc", bi)], writes=[("acc", bi)])

                    if nexp > 0:
                        load_w(0, 0)
                        U(0)
                    for it_i in range(len(items)):
                        ex_i, gi = items[it_i]
                        if gi == 0 and ex_i + 1 < nexp:
                            load_w(ex_i + 1, 1 - ex_i % 2)
                        if it_i + 1 < len(items):
                            U(it_i + 1)
                        Dn(it_i)
                    chk(9)
                    for bi, (srcT, rtile, which, dstT, drow) in enumerate(blk):
                        mT = modT[:, which, :, :]
                        E.dma("sp", xt[:], srcT[rtile * 128:(rtile + 1) * 128, :], writes=["xt"])
                        E.op("dve", lambda e: e.tensor_tensor(out=tmpf[:], in0=acc[:, bi, :], in1=mT[:, 2, :], op=ALU.mult), reads=[("acc", bi), "modT"], writes=["tmpf"])
                        E.op("pool", lambda e: e.tensor_tensor(out=tmpf[:], in0=tmpf[:], in1=xt[:], op=ALU.add), reads=["tmpf", "xt"], writes=["tmpf"])
                        if last:
                            E.op("pool", lambda e: e.memset(ssq[:], 0.0), writes=["ssq"])
                            E.op("act", lambda e: e.activation(out=h2[:], in_=tmpf[:], func=AF.Square, accum_out=ssq[:]), reads=["tmpf"], writes=["h2", "ssq"])
                            E.op("act", lambda e: e.activation(out=rt[:], in_=ssq[:], func=AF.Sqrt, scale=1.0 / D, bias=EPS), reads=["ssq"], writes=["rt"])
                            E.op("dve", lambda e: e.reciprocal(out=rt[:], in_=rt[:]), reads=["rt"], writes=["rt"])
                            E.op("dve", lambda e: e.scalar_tensor_tensor(out=h2[:], in0=tmpf[:], scalar=rt[:, 0:1], in1=fg[:], op0=ALU.mult, op1=ALU.mult),
                                 reads=["tmpf", "rt", "fg"], writes=["h2"])
                            E.dma("sp", dstT[drow * 128:(drow + 1) * 128, :], h2[:], reads=["h2"], writes=[("dram", id(dstT), drow)])
                        else:
                            E.dma("sp", dstT[drow * 128:(drow + 1) * 128, :], tmpf[:], reads=["tmpf"], writes=[("dram", id(dstT), drow)])
    except _Stop:
        pass
    E.dead = False
    E.barrier()
    es.close()
    return E


_CACHE = {}


def _get_nc():
    if "nc" not in _CACHE:
        nc0 = bass.Bass("TRN2", target_bir_lowering=False)
        E0 = build(nc0)
        nc = bass.Bass("TRN2", target_bir_lowering=False)
        build(nc, needed=E0.collected)
        _CACHE["nc"] = nc
    return _CACHE["nc"]


def _core_inputs(inputs):
    f32 = np.float32
    x = np.asarray(inputs["x"], f32); c = np.asarray(inputs["c"], f32)
    ctx = np.asarray(inputs["ctx"], f32); c_ctx = np.asarray(inputs["c_ctx"], f32)
    S = x.shape[1]
    shared = {k: np.ascontiguousarray(np.asarray(inputs[k], f32)) for k in (
        "norm1_g", "norm2_g", "ada_w", "ada_b", "w_in", "attn_sink", "sgu_ws", "sgu_b", "sgu_ln_g", "sgu_ln_b",
        "pool_w", "pool_scale", "conv_w", "w_out", "router_w", "router_b", "exp_w1", "exp_b1", "exp_w2", "exp_b2", "final_g")}
    jj = np.arange(128)
    masks = np.stack([(jj[:, None] >= jj[None, :]), (jj[:, None] <= jj[None, :])]).astype(f32)
    inv = (np.float32(10000.0) ** (-np.arange(16, dtype=f32) / np.float32(16))).astype(f32)
    wins = (2, 4, 8, 16)
    maps = []
    for core in range(8):
        b, r = core // 4, core % 4
        s = r * 4096
        xs = np.zeros((NT * 128, D), f32)
        lo, hi = s - 256, s + 4096 + 256
        a, e = max(lo, 0), min(hi, S)
        xs[a - lo:e - lo] = x[b, a:e]
        pos = (lo + np.arange(NT * 128)).astype(np.int64)
        row = (pos // 64).astype(f32); col = (pos % 64).astype(f32)
        ang = np.concatenate([row[:, None] * inv[None, :], col[:, None] * inv[None, :]], axis=1).astype(f32)
        pidx = (np.arange(128) % 64) % 32
        rope = np.stack([np.cos(ang).astype(f32)[:, pidx].T, np.sin(ang).astype(f32)[:, pidx].T]).astype(f32)
        flags = np.zeros((128, 4), f32)
        flags[:, 1] = 1.0 if r > 0 else 0.0
        flags[:, 2] = 1.0 if r < 3 else 0.0
        ptab = np.zeros((128, 5, 2, 128), f32)
        for p in range(128):
            for cc in range(2):
                w = wins[2 * cc + p // 64]
                ptab[p, 0, cc, :] = 1.0 / w
                for idx, (t0, N) in ((1, (s, S)), (2, (s + 31 * 128, S)), (3, (0, 256)), (4, (128, 256))):
                    t = t0 + np.arange(128)
                    cnt = np.clip(t + w - w // 2, 0, N) - np.clip(t - w // 2, 0, N)
                    ptab[p, idx, cc, :] = 1.0 / cnt.astype(f32)
        cvec = np.concatenate([c[b].reshape(8, 128).T, c_ctx.reshape(8, 128).T], axis=1).astype(f32)
        m = dict(shared)
        m.update(xs=xs, ctxb=np.ascontiguousarray(ctx[b]), cvec=np.ascontiguousarray(cvec), rope=np.ascontiguousarray(rope),
                 masks=masks, flags=flags, ptab=ptab)
        maps.append(m)
    return maps


def kernel(**inputs):
    nc = _get_nc()
    maps = _core_inputs(inputs)
    res = run_bass_kernel_spmd(nc, maps, core_ids=list(range(8)))
    x = inputs["x"]
    out = np.zeros(x.shape, np.float32)
    for core in range(8):
        b, r = core // 4, core % 4
        out[b, r * 4096:(r + 1) * 4096, :] = res.results[core]["y"]
    return out
```

```python
import numpy as np
from contextlib import ExitStack
import concourse.bass as bass
import concourse.mybir as mybir

F32 = mybir.dt.float32
BF16 = mybir.dt.bfloat16
AF = mybir.ActivationFunctionType
ALU = mybir.AluOpType
AX = mybir.AxisListType


class Emit:
    NDSEM = 10

    def __init__(self, nc, es, needed=None):
        self.nc = nc
        self.needed = needed
        self.collected = set()
        self.last_inc = {k: 0 for k in ("pe", "act", "dve", "pool")}
        self.eng = {"pe": nc.tensor, "act": nc.scalar, "dve": nc.vector,
                    "pool": nc.gpsimd, "sp": nc.sync}
        self.esem = {k: es.enter_context(nc.semaphore("e_" + k))
                     for k in ("pe", "act", "dve", "pool")}
        self.ecnt = {k: 0 for k in self.esem}
        self.dsem = {}
        self.dcnt = {}
        self.dnext = {}
        for q in ("sp", "act", "pool"):
            self.dsem[q] = [es.enter_context(nc.semaphore(f"d_{q}{i}"))
                            for i in range(self.NDSEM)]
            self.dnext[q] = 0
            for i in range(self.NDSEM):
                self.dcnt[(q, i)] = 0
        self.seen = {k: {} for k in self.eng}
        self.bufs = {}
        self.nwaits = 0
        self.nins = 0

    def _sem(self, key):
        if isinstance(key, tuple):
            return self.dsem[key[0]][key[1]]
        return self.esem[key]

    def _deps(self, reads, writes):
        deps = {}
        for k in reads:
            st = self.bufs.get(k)
            if st and st["w"]:
                s, v = st["w"]
                deps[s] = max(deps.get(s, 0), v)
        for k in writes:
            st = self.bufs.get(k)
            if st:
                if st["w"]:
                    s, v = st["w"]
                    deps[s] = max(deps.get(s, 0), v)
                for s, v in st["r"].items():
                    deps[s] = max(deps.get(s, 0), v)
        return deps

    def _wait(self, e, deps):
        seen = self.seen[e]
        for s, v in deps.items():
            if e == "pe" and s == "pe":
                continue
            if not isinstance(s, tuple):
                self.collected.add((s, v))
            if seen.get(s, 0) >= v:
                continue
            self.eng[e].wait_ge(self._sem(s), v)
            if not isinstance(s, tuple):
                self.collected.add((s, v))
            seen[s] = v
            self.nwaits += 1

    def _record(self, tok, reads, writes):
        s, v = tok
        for k in reads:
            st = self.bufs.setdefault(k, {"w": None, "r": {}})
            st["r"][s] = max(st["r"].get(s, 0), v)
        for k in writes:
            st = self.bufs.setdefault(k, {"w": None, "r": {}})
            st["w"] = tok
            st["r"] = {}

    dead = False

    def op(self, e, fn, reads=(), writes=()):
        if self.dead:
            return None
        self._wait(e, self._deps(reads, writes))
        ins = fn(self.eng[e])
        self.ecnt[e] += 1
        c = self.ecnt[e]
        if self.needed is None or (e, c) in self.needed:
            ins.then_inc(self.esem[e], c - self.last_inc[e])
            self.last_inc[e] = c
        self._record((e, c), reads, writes)
        self.nins += 1
        return ins

    def dma(self, q, out, in_, reads=(), writes=(), **kw):
        if self.dead:
            return None
        i = self.dnext[q]
        self.dnext[q] = (i + 1) % self.NDSEM
        key = (q, i)
        deps = self._deps(reads, writes)
        deps[key] = max(deps.get(key, 0), self.dcnt[key])
        self._wait(q, deps)
        ins = self.eng[q].dma_start(out=out, in_=in_, **kw)
        self.dcnt[key] += 16
        ins.then_inc(self.dsem[q][i], 16)
        self._record((key, self.dcnt[key]), reads, writes)
        self.nins += 1
        return ins

    def wait_all(self, e, keys):
        deps = self._deps(keys, ())
        self._wait(e, deps)

    def barrier(self):
        if self.dead:
            return
        deps = {k: self.ecnt[k] for k in self.esem}
        deps.update({k: v for k, v in self.dcnt.items()})
        for e in self.eng:
            self._wait(e, dict(deps))

from concourse.bass_utils import run_bass_kernel_spmd

NT = 36
D = 1024
EPS = 1e-6
C_Q, C_QR, C_K, C_KR, C_SU, C_PO, C_CB, C_CC, C_CX, C_VSV = 0, 512, 1024, 1152, 1280, 1536, 1792, 2048, 2304, 2560
NWC = 2944


class _Stop(Exception):
    pass


def build(nc, nlayers=2, dbg=False, nexp=32, stop=0, nblk=99, needed=None):
    es = ExitStack()
    E = Emit(nc, es, needed)

    def din(name, shape, dt=F32):
        return nc.dram_tensor(name, list(shape), dt, kind="ExternalInput").ap()

    xs = din("xs", [NT * 128, D]); ctxb = din("ctxb", [256, D]); cvec = din("cvec", [128, 16])
    rope = din("rope", [2, 128, NT * 128]); masks = din("masks", [2, 128, 128]); flags = din("flags", [128, 4])
    ptab = din("ptab", [128, 5, 2, 128])
    norm1_g = din("norm1_g", [2, D]); norm2_g = din("norm2_g", [2, D])
    ada_w = din("ada_w", [2, D, 6 * D]); ada_b = din("ada_b", [2, 6 * D])
    w_in = din("w_in", [2, D, 2304]); attn_sink = din("attn_sink", [2, 8])
    sgu_ws = din("sgu_ws", [2, 4, 128, 128]); sgu_b = din("sgu_b", [2, 4, 128])
    sgu_ln_g = din("sgu_ln_g", [2, 256]); sgu_ln_b = din("sgu_ln_b", [2, 256])
    pool_w = din("pool_w", [2, 4, 64, 64]); pool_scale = din("pool_scale", [2, 256])
    conv_w = din("conv_w", [2, 3, 256]); w_out = din("w_out", [2, 1280, D])
    router_w = din("router_w", [2, D, 32]); router_b = din("router_b", [2, 32])
    exp_w1 = din("exp_w1", [2, 32, D, 2048]); exp_b1 = din("exp_b1", [2, 32, 2048])
    exp_w2 = din("exp_w2", [2, 32, D, D]); exp_b2 = din("exp_b2", [2, 32, D])
    final_g = din("final_g", [D])
    yout = nc.dram_tensor("y", [32 * 128, D], F32, kind="ExternalOutput").ap()
    xm_s = nc.dram_tensor("xm_s", [NT * 128, D], F32, kind="ExternalOutput" if dbg else "Internal").ap()
    x1_s = nc.dram_tensor("x1_s", [NT * 128, D], F32, kind="Internal").ap()
    xcm_s = nc.dram_tensor("xcm_s", [256, D], F32, kind="Internal").ap()
    xc1_s = nc.dram_tensor("xc1_s", [256, D], F32, kind="Internal").ap()

    def SB(st, name, shape, dt=F32):
        return st.enter_context(nc.sbuf_tensor(name, list(shape), dt))

    def PS(name, shape, dt=F32):
        return es.enter_context(nc.psum_tensor(name, list(shape), dt))

    pA = PS("pA", [128, 512]); pB = PS("pB", [128, 512]); pC = PS("pC", [128, 512])
    pT = PS("pT", [128, 8, 128], BF16)
    pO = PS("pO", [128, 512]); pDen = PS("pDen", [128, 512])
    pW = [PS("pW0", [128, 512]), PS("pW1", [128, 512])]
    PN = {id(pA): "pA", id(pB): "pB", id(pC): "pC", id(pO): "pO", id(pDen): "pDen"}

    G = es
    ident_f = SB(G, "ident_f", [128, 128]); ident_b = SB(G, "ident_b", [128, 128], BF16)
    ones_f = SB(G, "ones_f", [128, 128]); ones_b = SB(G, "ones_b", [128, 128], BF16)
    mk = SB(G, "mk", [128, 4, 128], BF16)
    mkf = SB(G, "mkf", [128, 2, 128])
    flg = SB(G, "flg", [128, 4]); ptb = SB(G, "ptb", [128, 5, 2, 128])
    csv = SB(G, "csv", [128, 16])

    E.op("pool", lambda e: e.memset(ident_f[:], 1.0), writes=["ident_f"])
    E.op("pool", lambda e: e.affine_select(out=ident_f[:], in_=ident_f[:], pattern=[[-1, 128]],
                                           compare_op=ALU.is_equal, fill=0.0, base=0, channel_multiplier=1),
         reads=["ident_f"], writes=["ident_f"])
    E.op("pool", lambda e: e.tensor_copy(out=ident_b[:], in_=ident_f[:]), reads=["ident_f"], writes=["ident_b"])
    E.op("pool", lambda e: e.memset(ones_f[:], 1.0), writes=["ones_f"])
    E.op("pool", lambda e: e.memset(ones_b[:], 1.0), writes=["ones_b"])
    E.dma("sp", mkf[:], masks.rearrange("m p q -> p m q"), writes=["mkf"])
    E.dma("sp", flg[:], flags[:, :], writes=["flg"])
    E.dma("sp", ptb[:], ptab[:, :, :, :], writes=["ptb"])
    E.dma("sp", csv[:], cvec[:, :], writes=["csv"])
    E.op("dve", lambda e: e.tensor_copy(out=mk[:, 0:2, :], in_=mkf[:]), reads=["mkf"], writes=["mk"])
    E.op("dve", lambda e: e.tensor_scalar(out=mk[:, 2, :], in0=mkf[:, 0, :], scalar1=flg[:, 1:2], scalar2=None, op0=ALU.mult),
         reads=["mkf", "flg"], writes=["mk"])
    E.op("dve", lambda e: e.tensor_scalar(out=mk[:, 3, :], in0=mkf[:, 1, :], scalar1=flg[:, 2:3], scalar2=None, op0=ALU.mult),
         reads=["mkf", "flg"], writes=["mk"])
    E.op("act", lambda e: e.activation(out=csv[:], in_=csv[:], func=AF.Silu), reads=["csv"], writes=["csv"])

    uid = [0]

    def nm(s):
        uid[0] += 1
        return f"{s}_{uid[0]}"

    def bcast_load(st, dst, src_vec, key):
        E.dma("sp", dst, src_vec.partition_broadcast(128), writes=[key])

    def compute_mod(st, l, half, which, modT, tmp_stage, skey, csb, ckey2):
        for kc in range(8):
            E.op("pool", lambda e: e.tensor_copy(out=csb[:, kc, :], in_=csv[:, which * 8 + kc: which * 8 + kc + 1].to_broadcast([128, 128])),
                 reads=["csv"], writes=[ckey2])
        for nb in range(24):
            col0 = half * 3072 + nb * 128
            E.dma("sp", tmp_stage, ada_w[l, :, col0:col0 + 128].rearrange("(c p) n -> p c n", p=128), writes=[skey])
            sl = modT[:, nb // 8, (nb % 8) * 128:(nb % 8) * 128 + 128]
            bcast_load(st, sl, ada_b[l, col0:col0 + 128], "modT")
            for kc in range(8):
                E.op("pe", lambda e: e.matmul(pA[:, 0:128], lhsT=csb[:, kc, :], rhs=tmp_stage[:, kc, :], start=(kc == 0), stop=(kc == 7)),
                     reads=[ckey2, skey], writes=["pA"])
            E.op("dve", lambda e: e.tensor_tensor(out=sl, in0=pA[:, 0:128], in1=sl, op=ALU.add), reads=["pA", "modT"], writes=["modT"])

    def finish_mod(st, l, normg, modT, gts):
        for hf, (gap, gk) in enumerate(gts):
            bcast_load(st, gap, normg[l, hf * 512:(hf + 1) * 512], gk)
            E.op("dve", lambda e: e.scalar_tensor_tensor(out=modT[:, 1, hf * 512:(hf + 1) * 512], in0=modT[:, 1, hf * 512:(hf + 1) * 512], scalar=1.0, in1=gap, op0=ALU.add, op1=ALU.mult),
                 reads=["modT", gk], writes=["modT"])

    def norm_mod(xt, xkey, modT, out, okey, st_small, tmp, tmpkey):
        ssq, rt = st_small
        E.op("pool", lambda e: e.memset(ssq[:], 0.0), writes=["ssq"])
        E.op("act", lambda e: e.activation(out=tmp[:], in_=xt, func=AF.Square, accum_out=ssq[:]), reads=[xkey], writes=[tmpkey, "ssq"])
        E.op("act", lambda e: e.activation(out=rt[:], in_=ssq[:], func=AF.Sqrt, scale=1.0 / D, bias=EPS), reads=["ssq"], writes=["rt"])
        E.op("dve", lambda e: e.reciprocal(out=rt[:], in_=rt[:]), reads=["rt"], writes=["rt"])
        E.op("dve", lambda e: e.scalar_tensor_tensor(out=tmp[:], in0=xt, scalar=rt[:, 0:1], in1=modT[:, 1, :], op0=ALU.mult, op1=ALU.mult),
             reads=[xkey, "rt", "modT"], writes=[tmpkey])
        E.op("dve", lambda e: e.tensor_tensor(out=out, in0=tmp[:], in1=modT[:, 0, :], op=ALU.add), reads=[tmpkey, "modT"], writes=[okey])

    def chk(k):
        if stop == k:
            E.dead = True

    try:
        for l in range(nlayers):
            last = (l == nlayers - 1)
            xsrc = xs if l == 0 else x1_s
            csrc = ctxb if l == 0 else xc1_s
            p_lo, p_hi = (0, NT) if l == 0 else (1, NT - 1)
            m_lo, m_hi = (1, NT - 1) if l == 0 else (2, NT - 2)
            chk(1)
            E.barrier()
            with ExitStack() as ph:
                modT = SB(ph, nm("modT"), [128, 3, D])
                winb = SB(ph, nm("winb"), [128, 8, NWC], BF16)
                woA = SB(ph, nm("woA"), [64, 8, D], BF16); woB = SB(ph, nm("woB"), [128, 6, D], BF16)
                wsT = SB(ph, nm("wsT"), [128, 4, 128], BF16); bsT = SB(ph, nm("bsT"), [128, 2, 128])
                lng = SB(ph, nm("lng"), [128, 256]); lnb = SB(ph, nm("lnb"), [128, 256])
                PWm = SB(ph, nm("PW"), [128, 2, 128], BF16); PWf = SB(ph, nm("PWf"), [128, 2, 128])
                pscale = SB(ph, nm("pscale"), [128, 2]); cw = SB(ph, nm("cw"), [128, 2, 3])
                es8 = SB(ph, nm("es8"), [128, 8]); esink = SB(ph, nm("esink"), [64, 8, 128])
                kT = SB(ph, nm("kT"), [128, 3, 512], BF16); Vt = SB(ph, nm("V"), [128, 3, 4, 128], BF16)
                xpT = SB(ph, nm("xpT"), [128, 3, 2, 512], BF16); yT = SB(ph, nm("yT"), [128, 3, 2, 512], BF16)
                kcT = SB(ph, nm("kcT"), [128, 256], BF16); Vc = SB(ph, nm("Vc"), [128, 2, 128], BF16)
                qT = SB(ph, nm("qT"), [128, 2, 4, 512], BF16); suT = SB(ph, nm("suT"), [128, 2, 2, 512], BF16)
                cbT = SB(ph, nm("cbT"), [128, 2, 2, 512], BF16); ybT = SB(ph, nm("ybT"), [128, 2, 2, 512], BF16)
                ssq = SB(ph, nm("ssq"), [128, 1]); rt = SB(ph, nm("rt"), [128, 1])
                s1 = SB(ph, nm("s1"), [128, 1]); s2 = SB(ph, nm("s2"), [128, 1]); s3 = SB(ph, nm("s3"), [128, 1])
                tp = ph
                if True:
                    xt = SB(tp, nm("xt"), [128, D]); tmpf = SB(tp, nm("tmpf"), [128, D]); hb = SB(tp, nm("hb"), [128, D], BF16)
                    hT = SB(tp, nm("hT"), [128, 8, 512], BF16)
                    xr = SB(tp, nm("xr"), [128, D]); tmpM = SB(tp, nm("tmpM"), [128, D])
                    cs_t = SB(tp, nm("cs"), [128, 2, 512]); sn_t = SB(tp, nm("sn"), [128, 2, 512])
                    t1 = SB(tp, nm("t1"), [128, 512]); t2 = SB(tp, nm("t2"), [128, 512])
                    svg = SB(tp, nm("svg"), [128, 256]); vn = SB(tp, nm("vn"), [128, 256]); vnz = SB(tp, nm("vnz"), [128, 4, 128], BF16)
                    pexp = SB(tp, nm("pexp"), [128, 2, 512], BF16)
                    den = SB(tp, nm("den"), [64, 512]); attnT = SB(tp, nm("attnT"), [64, 2, 4, 128], BF16)
                    Xw = SB(tp, nm("Xw"), [128, 2, 160]); A2 = SB(tp, nm("A2"), [128, 2, 160]); A4 = SB(tp, nm("A4"), [128, 2, 160])
                    A8 = SB(tp, nm("A8"), [128, 2, 160]); A16 = SB(tp, nm("A16"), [128, 2, 160]); pd = SB(tp, nm("pd"), [128, 2, 128])
                    dT = SB(tp, nm("dT"), [128, 2, 128], BF16); ycT = SB(tp, nm("ycT"), [128, 2, 128], BF16)
                    Yw = SB(tp, nm("Yw"), [128, 2, 130]); zc = SB(tp, nm("zc"), [128, 2, 128]); ydT = SB(tp, nm("ydT"), [128, 2, 128], BF16)
                stage_ap = tmpf[:].rearrange("p (c n) -> p c n", c=8)
                csb_ap = xt[:].rearrange("p (c n) -> p c n", c=8)
                gts = [(t1[:], "t1"), (t2[:], "t2")]
                E.op("pool", lambda e: e.memset(cs_t[:, 1, :], 1.0), writes=[("cs", 1)])
                E.op("pool", lambda e: e.memset(sn_t[:, 1, :], 0.0), writes=[("cs", 1)])

                with ExitStack() as sg:
                    stg = SB(sg, nm("stg"), [128, 1, 1152])
                    for kc in range(8):
                        wk = ("winb", kc)
                        wb_ = winb[:, kc, :]
                        sk = ("stg", 0)
                        s_ = stg[:, 0, :]
                        E.dma("sp", s_, w_in[l, kc * 128:(kc + 1) * 128, 1152:2304], writes=[sk])
                        E.op("dve", lambda e: e.tensor_copy(out=wb_[:, C_PO:C_PO + 1024], in_=s_[:, 128:1152]), reads=[sk], writes=[wk])
                        E.op("pool", lambda e: e.tensor_copy(out=wb_[:, C_VSV + 256:C_VSV + 384], in_=s_[:, 0:128]), reads=[sk], writes=[wk])
                        sk = ("stg", 0)
                        s_ = stg[:, 0, :]
                        E.dma("sp", s_, w_in[l, kc * 128:(kc + 1) * 128, 0:1152], writes=[sk])
                        qv = s_[:, 0:512].rearrange("p (a j d) -> p j a d", a=2, j=4)
                        E.op("pool", lambda e: e.tensor_copy(out=wb_[:, C_Q:C_Q + 512].rearrange("p (j a d) -> p j a d", j=4, a=2), in_=qv),
                             reads=[sk], writes=[wk])
                        qvh = s_[:, 0:512].rearrange("p (a j t d) -> p j a t d", a=2, j=4, t=2)
                        qro = wb_[:, C_QR:C_QR + 512].rearrange("p (j a t d) -> p j a t d", j=4, a=2, t=2)
                        E.op("act", lambda e: e.mul(out=qro[:, :, :, 0, :], in_=qvh[:, :, :, 1, :], mul=-1.0), reads=[sk], writes=[wk])
                        E.op("pool", lambda e: e.tensor_copy(out=qro[:, :, :, 1, :], in_=qvh[:, :, :, 0, :]), reads=[sk], writes=[wk])
                        E.op("pool", lambda e: e.tensor_copy(out=wb_[:, C_K:C_K + 128], in_=s_[:, 512:640]), reads=[sk], writes=[wk])
                        kvh = s_[:, 512:640].rearrange("p (a t d) -> p a t d", a=2, t=2)
                        kro = wb_[:, C_KR:C_KR + 128].rearrange("p (a t d) -> p a t d", a=2, t=2)
                        E.op("act", lambda e: e.mul(out=kro[:, :, 0, :], in_=kvh[:, :, 1, :], mul=-1.0), reads=[sk], writes=[wk])
                        E.op("pool", lambda e: e.tensor_copy(out=kro[:, :, 1, :], in_=kvh[:, :, 0, :]), reads=[sk], writes=[wk])
                        E.op("act", lambda e: e.copy(out=wb_[:, C_SU:C_SU + 256], in_=s_[:, 768:1024]), reads=[sk], writes=[wk])
                        E.op("act", lambda e: e.copy(out=wb_[:, C_VSV:C_VSV + 128], in_=s_[:, 640:768]), reads=[sk], writes=[wk])
                        E.op("pool", lambda e: e.tensor_copy(out=wb_[:, C_VSV + 128:C_VSV + 256], in_=s_[:, 1024:1152]), reads=[sk], writes=[wk])
                    for hd in range(8):
                        sk = ("stg", 0)
                        sv_ = stg[0:64, 0, 0:1024]
                        E.dma("sp", sv_, w_out[l, hd * 64:(hd + 1) * 64, :], writes=[sk])
                        E.op("dve", lambda e: e.tensor_copy(out=woA[:, hd, :], in_=sv_), reads=[sk], writes=["woA"])
                    for c in range(6):
                        sk = ("stg", 0)
                        s_ = stg[:, 0, 0:1024]
                        E.dma("sp", s_, w_out[l, 512 + c * 128:512 + (c + 1) * 128, :], writes=[sk])
                        E.op("pool" if c % 2 else "dve", lambda e: e.tensor_copy(out=woB[:, c, :], in_=s_), reads=[sk], writes=["woB"])
                    for h in range(4):
                        sk = ("stg", 0)
                        s_ = stg[:, 0, 0:128]
                        E.dma("sp", s_, sgu_ws[l, h, :, :], writes=[sk])
                        E.op("pe", lambda e: e.transpose(out=pA[:, 0:128], in_=s_, identity=ident_f[:]), reads=[sk, "ident_f"], writes=["pA"])
                        E.op("act", lambda e: e.copy(out=wsT[:, h, :], in_=pA[:, 0:128]), reads=["pA"], writes=["wsT"])
                        E.dma("sp", bsT[(h % 2) * 64:(h % 2) * 64 + 64, h // 2, :], sgu_b[l, h, :].partition_broadcast(64), writes=["bsT"])
                    bcast_load(ph, lng[:], sgu_ln_g[l, :], "lng"); bcast_load(ph, lnb[:], sgu_ln_b[l, :], "lnb")
                    E.op("pool", lambda e: e.memset(PWf[:], 0.0), writes=["PWf"])
                    for g4 in range(4):
                        E.dma("sp", PWf[(g4 % 2) * 64:(g4 % 2) * 64 + 64, g4 // 2, (g4 % 2) * 64:(g4 % 2) * 64 + 64], pool_w[l, g4, :, :], reads=[], writes=["PWf"])
                    E.op("pool", lambda e: e.tensor_copy(out=PWm[:], in_=PWf[:]), reads=["PWf"], writes=["PW"])
                    for c in range(2):
                        E.dma("sp", pscale[:, c:c + 1], pool_scale[l, c * 128:(c + 1) * 128].rearrange("(p o) -> p o", o=1), writes=["pscale"])
                        for j in range(3):
                            E.dma("sp", cw[:, c, j:j + 1], conv_w[l, j, c * 128:(c + 1) * 128].rearrange("(p o) -> p o", o=1), writes=["cw"])
                    bcast_load(ph, es8[:], attn_sink[l, :], "es8")
                    E.op("act", lambda e: e.activation(out=es8[:], in_=es8[:], func=AF.Exp), reads=["es8"], writes=["es8"])
                    E.op("pool", lambda e: e.tensor_copy(out=esink[:], in_=es8[0:64, :].unsqueeze(2).to_broadcast([64, 8, 128])), reads=["es8"], writes=["esink"])

                    chk(2)
                    compute_mod(sg, l, 0, 1, modT, stage_ap, "tmpf", csb_ap, "xt")
                    finish_mod(sg, l, norm1_g, modT, gts)
                E.barrier()
                with ExitStack() as tp:
                    smallst = (ssq, rt)
                    E.op("pool", lambda e: e.memset(vnz[:], 0.0), writes=["vnz"])
                    rot = {"i": 0}

                    def proj_group(tiles, src, src_row0, slot, par, cosT, sinT, ckey, kdst, kkey, vdst, vkey, xpdst, xpkey, ydst, ykey, flag_tiles, hooks=()):
                        hooks = list(hooks)
                        for (ts, rtile, ti) in tiles:
                            if hooks:
                                hooks.pop(0)()
                            E.dma("act", xt[:], src[rtile * 128:(rtile + 1) * 128, :], writes=["xt"])
                            norm_mod(xt[:], "xt", modT, hb[:], "hb", smallst, tmpf, "tmpf")
                            for kc in range(8):
                                E.op("pe", lambda e: e.transpose(out=pT[:, kc, :], in_=hb[:, kc * 128:(kc + 1) * 128], identity=ident_b[:]),
                                     reads=["hb", "ident_b"], writes=["pT"])
                            E.op("act", lambda e: e.copy(out=hT[:, :, ts * 128:(ts + 1) * 128], in_=pT[:]), reads=["pT"], writes=[("hT", ts)])
                            for kc in range(8):
                                E.op("pe", lambda e: e.matmul(pC[:, 0:384], lhsT=hT[:, kc, ts * 128:(ts + 1) * 128], rhs=winb[:, kc, C_VSV:C_VSV + 384],
                                                              start=(kc == 0), stop=(kc == 7)), reads=[("hT", ts), ("winb", kc)], writes=["pC"])
                            E.op("dve", lambda e: e.tensor_copy(out=vdst[:, ts, :], in_=pC[:, 0:128]), reads=["pC"], writes=[vkey])
                            E.op("pool", lambda e: e.memset(s1[:], 0.0), writes=["s1"])
                            E.op("pool", lambda e: e.memset(s2[:], 0.0), writes=["s2"])
                            E.op("act", lambda e: e.activation(out=svg[:], in_=pC[:, 128:384], func=AF.Gelu, accum_out=s1[:]), reads=["pC"], writes=["svg", "s1"])
                            E.op("dve", lambda e: e.tensor_scalar(out=s1[:], in0=s1[:], scalar1=-1.0 / 256, scalar2=None, op0=ALU.mult), reads=["s1"], writes=["s1"])
                            E.op("act", lambda e: e.activation(out=vn[:], in_=svg[:], func=AF.Square, bias=s1[:, 0:1], accum_out=s2[:]), reads=["svg", "s1"], writes=["vn", "s2"])
                            E.op("act", lambda e: e.activation(out=s3[:], in_=s2[:], func=AF.Sqrt, scale=1.0 / 256, bias=EPS), reads=["s2"], writes=["s3"])
                            E.op("dve", lambda e: e.reciprocal(out=s3[:], in_=s3[:]), reads=["s3"], writes=["s3"])
                            E.op("dve", lambda e: e.tensor_scalar(out=vn[:], in0=svg[:], scalar1=s1[:, 0:1], scalar2=s3[:, 0:1], op0=ALU.add, op1=ALU.mult),
                                 reads=["svg", "s1", "s3"], writes=["vn"])
                            E.op("pool", lambda e: e.tensor_tensor(out=vn[:], in0=vn[:], in1=lng[:], op=ALU.mult), reads=["vn", "lng"], writes=["vn"])
                            vn4 = vn[:].rearrange("p (h d) -> p h d", h=4)
                            lb4 = lnb[:].rearrange("p (h d) -> p h d", h=4)
                            for par2 in range(2):
                                E.op("pool", lambda e: e.tensor_tensor(out=vnz[:, par2::2, par2 * 64:par2 * 64 + 64], in0=vn4[:, par2::2, :], in1=lb4[:, par2::2, :], op=ALU.add),
                                     reads=["vn", "lnb"], writes=["vnz"])
                            for c in range(2):
                                E.op("pe", lambda e: e.matmul(pB[:, c * 128:(c + 1) * 128], lhsT=vnz[:, 2 * c, :], rhs=wsT[:, 2 * c, :], start=True, stop=False),
                                     reads=["vnz", "wsT"], writes=["pB"])
                                E.op("pe", lambda e: e.matmul(pB[:, c * 128:(c + 1) * 128], lhsT=vnz[:, 2 * c + 1, :], rhs=wsT[:, 2 * c + 1, :], start=False, stop=True),
                                     reads=["vnz", "wsT"], writes=["pB"])
                            E.op("dve", lambda e: e.tensor_tensor(out=t1[:, 0:256].rearrange("p (c q) -> p c q", c=2), in0=pB[:, 0:256].rearrange("p (c q) -> p c q", c=2), in1=bsT[:], op=ALU.add),
                                 reads=["pB", "bsT"], writes=["t1"])
                            E.op("pool", lambda e: e.tensor_copy(out=ybT[:, par, :, ts * 128:(ts + 1) * 128], in_=t1[:, 0:256].rearrange("p (c q) -> p c q", c=2)),
                                 reads=["t1"], writes=[("ybT", par)])
                        while hooks:
                            hooks.pop(0)()
                        tsl = [t[0] for t in tiles]
                        c0, c1 = min(tsl) * 128, (max(tsl) + 1) * 128
                        n = c1 - c0
                        hkeys = [("hT", t) for t in tsl]

                        def fm(col, pp):
                            for kc in range(8):
                                E.op("pe", lambda e: e.matmul(pp[:, 0:n], lhsT=winb[:, kc, col:col + 128], rhs=hT[:, kc, c0:c1], start=(kc == 0), stop=(kc == 7)),
                                     reads=hkeys + [("winb", kc)], writes=[PN[id(pp)]])
                        for j in range(5):
                            ca, cb_ = (C_Q + j * 128, C_QR + j * 128) if j < 4 else (C_K, C_KR)
                            fm(ca, pA); fm(cb_, pB)
                            E.op("dve", lambda e: e.tensor_tensor(out=t1[:, 0:n], in0=pA[:, 0:n], in1=cosT[:, c0:c1], op=ALU.mult), reads=["pA", ckey], writes=["t1"])
                            E.op("dve", lambda e: e.tensor_tensor(out=t2[:, 0:n], in0=pB[:, 0:n], in1=sinT[:, c0:c1], op=ALU.mult), reads=["pB", ckey], writes=["t2"])
                            if j < 4:
                                E.op("dve", lambda e: e.tensor_tensor(out=qT[:, par, j, c0:c1], in0=t1[:, 0:n], in1=t2[:, 0:n], op=ALU.add), reads=["t1", "t2"], writes=[("qT", par)])
                            else:
                                E.op("dve", lambda e: e.tensor_tensor(out=kdst[:, c0:c1], in0=t1[:, 0:n], in1=t2[:, 0:n], op=ALU.add), reads=["t1", "t2"], writes=[kkey])
                        for c in range(2):
                            fm(C_SU + c * 128, pA)
                            E.op("act", lambda e: e.activation(out=suT[:, par, c, c0:c1], in_=pA[:, 0:n], func=AF.Gelu), reads=["pA"], writes=[("suT", par)])
                            fm(C_PO + c * 128, pB)
                            E.op("act", lambda e: e.copy(out=xpdst[:, c, c0:c1], in_=pB[:, 0:n]), reads=["pB"], writes=[xpkey])
                            fm(C_CB + c * 128, pA)
                            E.op("act", lambda e: e.copy(out=cbT[:, par, c, c0:c1], in_=pA[:, 0:n]), reads=["pA"], writes=[("cbT", par)])
                            fm(C_CC + c * 128, pA); fm(C_CX + c * 128, pB)
                            E.op("act", lambda e: e.copy(out=t1[:, 0:n], in_=pA[:, 0:n]), reads=["pA"], writes=["t1"])
                            E.op("dve", lambda e: e.tensor_tensor(out=ydst[:, c, c0:c1], in0=t1[:, 0:n], in1=pB[:, 0:n], op=ALU.mult), reads=["t1", "pB"], writes=[ykey])
                        E.op("pool", lambda e: e.tensor_tensor(out=ybT[:, par, :, c0:c1], in0=ybT[:, par, :, c0:c1], in1=suT[:, par, :, c0:c1], op=ALU.mult),
                             reads=[("ybT", par), ("suT", par)], writes=[("ybT", par)])
                        for (ts, fcol) in flag_tiles:
                            E.op("pool", lambda e: e.tensor_scalar(out=xpdst[:, :, ts * 128:(ts + 1) * 128], in0=xpdst[:, :, ts * 128:(ts + 1) * 128], scalar1=flg[:, fcol:fcol + 1], scalar2=None, op0=ALU.mult),
                                 reads=[xpkey, "flg"], writes=[xpkey])
                            E.op("pool", lambda e: e.tensor_scalar(out=ydst[:, :, ts * 128:(ts + 1) * 128], in0=ydst[:, :, ts * 128:(ts + 1) * 128], scalar1=flg[:, fcol:fcol + 1], scalar2=None, op0=ALU.mult),
                                 reads=[ykey, "flg"], writes=[ykey])

                    def mix_tile(ts, par, keyblocks, winX, winY, tabidx, src, rtile, gmod, dst, drow):
                        qc0 = ts * 128
                        E.dma("act", xr[:], src[rtile * 128:(rtile + 1) * 128, :], writes=["xr"])
                        for gg in range(2):
                            pr = slice(gg * 64, gg * 64 + 64)
                            for bi, (kv_, kk_, vv_, vk_, mi) in enumerate(keyblocks):
                                pS = pA if bi % 2 == 0 else pB
                                E.op("pe", lambda e: e.matmul(pS[:].rearrange("p (j q) -> p j q", j=4), lhsT=kv_[pr, :], rhs=qT[pr, par, :, qc0:qc0 + 128], start=True, stop=True),
                                     reads=[kk_, ("qT", par)], writes=[PN[id(pS)]])
                                pe_ = pexp[:, bi % 2, :]
                                pk = ("pexp", bi % 2)
                                E.op("act", lambda e: e.activation(out=pe_, in_=pS[:], func=AF.Exp, scale=0.125), reads=[PN[id(pS)]], writes=[pk])
                                if mi is not None:
                                    E.op("dve", lambda e: e.tensor_tensor(out=pe_.rearrange("p (j q) -> p j q", j=4), in0=pe_.rearrange("p (j q) -> p j q", j=4),
                                                                            in1=mk[:, mi, :].unsqueeze(1).to_broadcast([128, 4, 128]), op=ALU.mult), reads=[pk, "mk"], writes=[pk])
                                first, lastb = (bi == 0), (bi == len(keyblocks) - 1)
                                E.op("pe", lambda e: e.matmul(pO[0:64, :], lhsT=vv_[:, pr], rhs=pe_, start=first, stop=lastb), reads=[vk_, pk], writes=["pO"])
                                E.op("pe", lambda e: e.matmul(pDen[0:64, :], lhsT=ones_b[:, 0:64], rhs=pe_, start=first, stop=lastb), reads=["ones_b", pk], writes=["pDen"])
                            E.op("dve", lambda e: e.tensor_tensor(out=den[:].rearrange("p (j q) -> p j q", j=4), in0=pDen[0:64, :].rearrange("p (j q) -> p j q", j=4), in1=esink[:, gg * 4:gg * 4 + 4, :], op=ALU.add),
                                 reads=["pDen", "esink"], writes=["den"])
                            E.op("dve", lambda e: e.reciprocal(out=den[:], in_=den[:]), reads=["den"], writes=["den"])
                            E.op("dve", lambda e: e.tensor_tensor(out=attnT[:, gg, :, :], in0=pO[0:64, :].rearrange("p (j q) -> p j q", j=4), in1=den[:].rearrange("p (j q) -> p j q", j=4), op=ALU.mult),
                                 reads=["pO", "den"], writes=["attnT"])
                        for (d0, ap_, k_) in winX:
                            nn = ap_.shape[-1]
                            E.op("pool", lambda e: e.tensor_copy(out=Xw[:, :, d0:d0 + nn], in_=ap_), reads=[k_], writes=["Xw"])
                        E.op("pool", lambda e: e.tensor_tensor(out=A2[:, :, 1:160], in0=Xw[:, :, 1:160], in1=Xw[:, :, 0:159], op=ALU.add), reads=["Xw"], writes=["A2"])
                        E.op("pool", lambda e: e.tensor_tensor(out=A4[:, :, 2:159], in0=A2[:, :, 3:160], in1=A2[:, :, 1:158], op=ALU.add), reads=["A2"], writes=["A4"])
                        E.op("pool", lambda e: e.tensor_tensor(out=A8[:, :, 4:157], in0=A4[:, :, 6:159], in1=A4[:, :, 2:155], op=ALU.add), reads=["A4"], writes=["A8"])
                        E.op("pool", lambda e: e.tensor_tensor(out=A16[:, :, 8:153], in0=A8[:, :, 12:157], in1=A8[:, :, 4:149], op=ALU.add), reads=["A8"], writes=["A16"])
                        for (c, half, lev, lk) in ((0, 0, A2, "A2"), (0, 1, A4, "A4"), (1, 0, A8, "A8"), (1, 1, A16, "A16")):
                            pr = slice(half * 64, half * 64 + 64)
                            E.op("dve", lambda e: e.tensor_tensor(out=pd[pr, c, :], in0=lev[pr, c, 16:144], in1=ptb[pr, tabidx, c, :], op=ALU.mult), reads=[lk, "ptb"], writes=["pd"])
                        E.op("pool", lambda e: e.tensor_tensor(out=dT[:], in0=pd[:], in1=Xw[:, :, 16:144], op=ALU.subtract), reads=["pd", "Xw"], writes=["dT"])
                        for c in range(2):
                            E.op("pe", lambda e: e.matmul(pC[:, c * 128:(c + 1) * 128], lhsT=PWm[:, c, :], rhs=dT[:, c, :], start=True, stop=True), reads=["PW", "dT"], writes=["pC"])
                            E.op("dve", lambda e: e.tensor_scalar(out=ycT[:, c, :], in0=pC[:, c * 128:(c + 1) * 128], scalar1=pscale[:, c:c + 1], scalar2=None, op0=ALU.mult),
                                 reads=["pC", "pscale"], writes=["ycT"])
                        for (d0, ap_, k_) in winY:
                            nn = ap_.shape[-1]
                            E.op("pool", lambda e: e.tensor_copy(out=Yw[:, :, d0:d0 + nn], in_=ap_), reads=[k_], writes=["Yw"])
                        for c in range(2):
                            E.op("dve", lambda e: e.tensor_scalar(out=zc[:, c, :], in0=Yw[:, c, 0:128], scalar1=cw[:, c, 0:1], scalar2=None, op0=ALU.mult), reads=["Yw", "cw"], writes=["zc"])
                            E.op("dve", lambda e: e.scalar_tensor_tensor(out=zc[:, c, :], in0=Yw[:, c, 1:129], scalar=cw[:, c, 1:2], in1=zc[:, c, :], op0=ALU.mult, op1=ALU.add), reads=["Yw", "cw", "zc"], writes=["zc"])
                            E.op("dve", lambda e: e.scalar_tensor_tensor(out=zc[:, c, :], in0=Yw[:, c, 2:130], scalar=cw[:, c, 2:3], in1=zc[:, c, :], op0=ALU.mult, op1=ALU.add), reads=["Yw", "cw", "zc"], writes=["zc"])
                        E.op("pool", lambda e: e.tensor_tensor(out=ydT[:], in0=zc[:], in1=cbT[:, par, :, qc0:qc0 + 128], op=ALU.mult), reads=["zc", ("cbT", par)], writes=["ydT"])
                        for nh in range(2):
                            ns = slice(nh * 512, nh * 512 + 512)
                            for hd in range(8):
                                E.op("pe", lambda e: e.matmul(pW[nh][:], lhsT=attnT[:, hd // 4, hd % 4, :], rhs=woA[:, hd, ns], start=(hd == 0), stop=False), reads=["attnT", "woA"], writes=[("pW", nh)])
                            srcs = [(ybT[:, par, 0, qc0:qc0 + 128], ("ybT", par)), (ybT[:, par, 1, qc0:qc0 + 128], ("ybT", par)),
                                    (ycT[:, 0, :], "ycT"), (ycT[:, 1, :], "ycT"), (ydT[:, 0, :], "ydT"), (ydT[:, 1, :], "ydT")]
                            for c, (ap_, k_) in enumerate(srcs):
                                E.op("pe", lambda e: e.matmul(pW[nh][:], lhsT=ap_, rhs=woB[:, c, ns], start=False, stop=(c == 5)), reads=[k_, "woB"], writes=[("pW", nh)])
                        for nh in range(2):
                            ns = slice(nh * 512, nh * 512 + 512)
                            E.op("dve", lambda e: e.tensor_tensor(out=tmpM[:, ns], in0=pW[nh][:], in1=gmod[:, 2, ns], op=ALU.mult), reads=[("pW", nh), "modT"], writes=["tmpM"])
                        E.op("dve", lambda e: e.tensor_tensor(out=tmpM[:], in0=tmpM[:], in1=xr[:], op=ALU.add), reads=["tmpM", "xr"], writes=["tmpM"])
                        E.dma("sp", dst[drow * 128:(drow + 1) * 128, :], tmpM[:], reads=["tmpM"], writes=[("dram", id(dst), drow)])

                    xpC = SB(tp, nm("xpC"), [128, 2, 256], BF16); yC = SB(tp, nm("yC"), [128, 2, 256], BF16)
                    zpad = SB(tp, nm("zpad"), [128, 2, 16], BF16)
                    E.op("pool", lambda e: e.memset(zpad[:], 0.0), writes=["zpad"])
                    chk(3)
                    ctiles = [(0, 0, 0), (1, 1, 1)]
                    proj_group(ctiles, csrc, 0, None, 1, cs_t[:, 1, :], sn_t[:, 1, :], ("cs", 1), kcT, "kcT", Vc, "Vc", xpC, "xpC", yC, "yC", [])
                    chk(4)
                    ctxblocks = [(kcT[:, 0:128], "kcT", Vc[:, 0, :], "Vc", None), (kcT[:, 128:256], "kcT", Vc[:, 1, :], "Vc", None)]
                    if not last:
                        for ts in range(2):
                            wx = [(0, zpad[:, :, 0:16], "zpad") if ts == 0 else (0, xpC[:, :, 112:128], "xpC"),
                                  (16, xpC[:, :, ts * 128:(ts + 1) * 128], "xpC"),
                                  (144, xpC[:, :, 128:144], "xpC") if ts == 0 else (144, zpad[:, :, 0:16], "zpad")]
                            wy = [(0, zpad[:, :, 0:1], "zpad") if ts == 0 else (0, yC[:, :, 127:128], "yC"),
                                  (1, yC[:, :, ts * 128:(ts + 1) * 128], "yC"),
                                  (129, yC[:, :, 128:129], "yC") if ts == 0 else (129, zpad[:, :, 0:1], "zpad")]
                            mix_tile(ts, 1, ctxblocks, wx, wy, 3 + ts, csrc, ts, modT, xcm_s, ts)
                    chk(5)
                    compute_mod(None, l, 0, 0, modT, stage_ap, "tmpf", csb_ap, "xt")
                    finish_mod(None, l, norm1_g, modT, gts)
                    E.barrier()

                    ngroups = NT // 4

                    def P(g, hooks=()):
                        tiles = [(ts, g * 4 + ts, g * 4 + ts) for ts in range(4) if p_lo <= g * 4 + ts < p_hi]
                        slot = g % 3
                        par = g % 2
                        E.dma("act", cs_t[:, par, :], rope[0, :, g * 512:(g + 1) * 512], writes=[("cs", par)])
                        E.dma("act", sn_t[:, par, :], rope[1, :, g * 512:(g + 1) * 512], writes=[("cs", par)])
                        fl = []
                        if g == 0:
                            fl.append((1, 1))
                        if g == ngroups - 1:
                            fl.append((2, 2))
                        proj_group(tiles, xsrc, 0, slot, par, cs_t[:, par, :], sn_t[:, par, :], ("cs", par),
                                   kT[:, slot, :], ("kT", slot), Vt[:, slot, :, :], ("V", slot),
                                   xpT[:, slot, :, :], ("xpT", slot), yT[:, slot, :, :], ("yT", slot), fl, hooks)

                    def M(g, tss):
                        par = g % 2
                        for ts in tss:
                            ti = g * 4 + ts

                            def tl(t):
                                return (t // 4) % 3, t % 4
                            blocks = []
                            for t, mi in ((ti - 1, 2 if ti == 2 else 0), (ti, None), (ti + 1, 3 if ti == NT - 3 else 1)):
                                sl, tt = tl(t)
                                blocks.append((kT[:, sl, tt * 128:(tt + 1) * 128], ("kT", sl), Vt[:, sl, tt, :], ("V", sl), mi))
                            blocks += ctxblocks
                            (sp_, tp_), (sc_, tc_), (sn_, tn_) = tl(ti - 1), tl(ti), tl(ti + 1)
                            wx = [(0, xpT[:, sp_, :, tp_ * 128 + 112:tp_ * 128 + 128], ("xpT", sp_)),
                                  (16, xpT[:, sc_, :, tc_ * 128:(tc_ + 1) * 128], ("xpT", sc_)),
                                  (144, xpT[:, sn_, :, tn_ * 128:tn_ * 128 + 16], ("xpT", sn_))]
                            wy = [(0, yT[:, sp_, :, tp_ * 128 + 127:tp_ * 128 + 128], ("yT", sp_)),
                                  (1, yT[:, sc_, :, tc_ * 128:(tc_ + 1) * 128], ("yT", sc_)),
                                  (129, yT[:, sn_, :, tn_ * 128:tn_ * 128 + 1], ("yT", sn_))]
                            tabidx = 1 if ti == 2 else (2 if ti == NT - 3 else 0)
                            mix_tile(ts, par, blocks, wx, wy, tabidx, xsrc, ti, modT, xm_s, ti)

                    chk(6)
                    P(0)
                    for g in range(ngroups):
                        mt = [ts for ts in range(4) if m_lo <= g * 4 + ts < m_hi]
                        if g + 1 < ngroups:
                            early = [ts for ts in mt if ts < 3]
                            late = [ts for ts in mt if ts == 3]
                            P(g + 1, [(lambda ts=ts, g=g: M(g, [ts])) for ts in early])
                            M(g, late)
                        else:
                            M(g, mt)
            chk(7)
            E.barrier()
            with ExitStack() as ph:
                modT = SB(ph, nm("modT2"), [128, 1 if last else 2, 3, D])
                chk(71)
                fg = SB(ph, nm("fg"), [128, D if last else 1])
                if last:
                    bcast_load(ph, fg[:], final_g, "fg")
                rw = SB(ph, nm("rw"), [128, 8, 32]); rb = SB(ph, nm("rb"), [1, 32])
                E.dma("sp", rw[:], router_w[l].rearrange("(c p) n -> p c n", p=128), writes=["rw"])
                E.dma("sp", rb[:], router_b[l:l + 1, :], writes=["rb"])
                TB = 7 if last else 6
                w1b = SB(ph, nm("w1b"), [128, 2, 8, 2048], BF16); w2b = SB(ph, nm("w2b"), [128, 2, 8, D], BF16)
                b1t = SB(ph, nm("b1t"), [128, 2, 8, 2]); GTb = SB(ph, nm("GTb"), [32, 128], BF16); B2b = SB(ph, nm("B2b"), [32, D], BF16)
                E.dma("pool", B2b[:], exp_b2[l, :, :], writes=["B2b"])
                h2T = SB(ph, nm("h2T"), [128, TB, 8, 128], BF16); acc = SB(ph, nm("acc"), [128, TB, D])
                Gt = SB(ph, nm("G"), [128, TB, 32])
                xt = SB(ph, nm("xt2"), [128, D]); tmpf = SB(ph, nm("tmpf2"), [128, D]); h2 = SB(ph, nm("h2"), [128, D])
                h2T32 = tmpf[:].rearrange("p (c n) -> p c n", c=8)
                lg = SB(ph, nm("lg"), [128, 32]); mx8 = SB(ph, nm("mx8"), [128, 8]); msk = SB(ph, nm("msk"), [128, 32])
                ex = SB(ph, nm("ex"), [128, 32]); sm = SB(ph, nm("sm"), [128, 1]); ngm = SB(ph, nm("ngm"), [128, 1])
                ssq = SB(ph, nm("ssq2"), [128, 1]); rt = SB(ph, nm("rt2"), [128, 1])
                gc = SB(ph, nm("gc"), [128, 2, 512]); sgm = h2[:].rearrange("p (s n) -> p s n", s=2); lc = SB(ph, nm("lc"), [128, 2, 512])
                aT = SB(ph, nm("aT"), [128, 2, 8, 512], BF16)
                pZ = [pA, pB, pC, pO]
                chk(72)
                for which in ((0, 1) if not last else (0,)):
                    compute_mod(None, l, 1, which, modT[:, which, :, :], tmpf[:].rearrange("p (c n) -> p c n", c=8), "tmpf", xt[:].rearrange("p (c n) -> p c n", c=8), "xt")
                    finish_mod(None, l, norm2_g, modT[:, which, :, :], [(h2[:, 0:512], "h2"), (h2[:, 512:1024], "h2")])
                E.barrier()
                pTf = pDen

                chk(725)
                if not last:
                    work = [(xm_s, ti, 0, x1_s, ti) for ti in range(1, NT - 1)] + [(xcm_s, t, 1, xc1_s, t) for t in range(2)]
                else:
                    work = [(xm_s, ti, 0, yout, ti - 2) for ti in range(2, NT - 2)]
                bsizes = [7, 7, 6, 6, 6] if last else [6] * 6
                assert sum(bsizes) == len(work)
                bstarts = [sum(bsizes[:i]) for i in range(len(bsizes))]
                for b0, bsz in list(zip(bstarts, bsizes))[:nblk]:
                    blk = work[b0:b0 + bsz]
                    for bi, (srcT, rtile, which, dstT, drow) in enumerate(blk):
                        mT = modT[:, which, :, :]
                        E.dma("sp", xt[:], srcT[rtile * 128:(rtile + 1) * 128, :], writes=["xt"])
                        chk(730)
                        norm_mod(xt[:], "xt", mT, h2[:], "h2", (ssq, rt), tmpf, "tmpf")
                        chk(731)
                        for half in range(2):
                            for j in range(4):
                                kc = half * 4 + j
                                E.op("pe", lambda e: e.transpose(out=pTf[:, j * 128:(j + 1) * 128], in_=h2[:, kc * 128:(kc + 1) * 128], identity=ident_f[:]),
                                     reads=["h2", "ident_f"], writes=["pDen"])
                            chk(732)
                            E.op("act", lambda e: e.copy(out=h2T32[:, half * 4:half * 4 + 4, :], in_=pTf[:].rearrange("p (j q) -> p j q", j=4)), reads=["pDen"], writes=["tmpf"])
                            chk(733)
                            E.op("act", lambda e: e.copy(out=h2T[:, bi, half * 4:half * 4 + 4, :], in_=pTf[:].rearrange("p (j q) -> p j q", j=4)), reads=["pDen"], writes=[("h2T", bi)])
                        chk(73)
                        for kc in range(8):
                            E.op("pe", lambda e: e.matmul(pW[0][:, 0:32], lhsT=h2T32[:, kc, :], rhs=rw[:, kc, :], start=(kc == 0), stop=False), reads=["tmpf", "rw"], writes=[("pW", 0)])
                        E.op("pe", lambda e: e.matmul(pW[0][:, 0:32], lhsT=ones_f[0:1, :], rhs=rb[0:1, :], start=False, stop=True), reads=["ones_f", "rb"], writes=[("pW", 0)])
                        chk(74)
                        E.op("dve", lambda e: e.tensor_copy(out=lg[:], in_=pW[0][:, 0:32]), reads=[("pW", 0)], writes=["lg"])
                        E.op("dve", lambda e: e.max(out=mx8[:], in_=lg[:]), reads=["lg"], writes=["mx8"])
                        E.op("dve", lambda e: e.tensor_scalar(out=msk[:], in0=lg[:], scalar1=mx8[:, 3:4], scalar2=None, op0=ALU.is_ge), reads=["lg", "mx8"], writes=["msk"])
                        chk(75)
                        E.op("dve", lambda e: e.tensor_scalar(out=ngm[:], in0=mx8[:, 0:1], scalar1=-1.0, scalar2=None, op0=ALU.mult), reads=["mx8"], writes=["ngm"])
                        E.op("act", lambda e: e.activation(out=ex[:], in_=lg[:], func=AF.Exp, bias=ngm[:, 0:1]), reads=["lg", "ngm"], writes=["ex"])
                        E.op("dve", lambda e: e.tensor_tensor(out=ex[:], in0=ex[:], in1=msk[:], op=ALU.mult), reads=["ex", "msk"], writes=["ex"])
                        E.op("dve", lambda e: e.reduce_sum(out=sm[:], in_=ex[:], axis=AX.X), reads=["ex"], writes=["sm"])
                        E.op("dve", lambda e: e.reciprocal(out=sm[:], in_=sm[:]), reads=["sm"], writes=["sm"])
                        E.op("dve", lambda e: e.tensor_scalar(out=Gt[:, bi, :], in0=ex[:], scalar1=sm[:, 0:1], scalar2=None, op0=ALU.mult), reads=["ex", "sm"], writes=[("G", bi)])
                        E.op("pe", lambda e: e.transpose(out=pTf[0:32, 0:128], in_=Gt[:, bi, :], identity=ident_f[:]), reads=[("G", bi), "ident_f"], writes=["pDen"])
                        E.op("act", lambda e: e.copy(out=GTb[:], in_=pTf[0:32, 0:128]), reads=["pDen"], writes=["GTb"])
                        for nh in range(2):
                            E.op("pe", lambda e: e.matmul(pW[nh][:], lhsT=GTb[:], rhs=B2b[:, nh * 512:(nh + 1) * 512], start=True, stop=True), reads=["GTb", "B2b"], writes=[("pW", nh)])
                            E.op("act", lambda e: e.copy(out=acc[:, bi, nh * 512:(nh + 1) * 512], in_=pW[nh][:]), reads=[("pW", nh)], writes=[("acc", bi)])
                    chk(8)
                    nb_t = len(blk)

                    def load_w(ex_i, wpar):
                        for kc in range(8):
                            E.dma("pool", w1b[:, wpar, kc, :], exp_w1[l, ex_i, kc * 128:(kc + 1) * 128, :], writes=[("w1b", wpar, kc)])
                        E.dma("sp", b1t[:, wpar, :, :], exp_b1[l, ex_i, :].rearrange("(c p t) -> p c t", p=128, t=2), writes=[("b1t", wpar)])
                        E.op("dve", lambda e: e.tensor_scalar(out=b1t[:, wpar, :, 1], in0=b1t[:, wpar, :, 1], scalar1=1.0, scalar2=None, op0=ALU.add), reads=[("b1t", wpar)], writes=[("b1t", wpar)])
                        for kc in range(0, 8, 2):
                            E.dma("pool", w2b[:, wpar, kc:kc + 2, :], exp_w2[l, ex_i, kc * 128:(kc + 2) * 128, :].rearrange("(c p) n -> p c n", p=128),
                                  writes=[("w2b", wpar, kc), ("w2b", wpar, kc + 1)])

                    groups = [(t0, min(4, nb_t - t0)) for t0 in range(0, nb_t, 4)]
                    items = [(ex_i, gi) for ex_i in range(nexp) for gi in range(len(groups))]

                    def U(it_i):
                        ex_i, gi = items[it_i]
                        wpar = ex_i % 2
                        ap = it_i % 2
                        t0, nt = groups[gi]
                        n = nt * 128
                        hk = [("h2T", t0 + t) for t in range(nt)]
                        for fc in range(8):
                            pg, pl_ = (pA, pB) if fc % 2 == 0 else (pC, pO)
                            s_ = fc % 2
                            for (pz, off) in ((pg, 0), (pl_, 1)):
                                for kc in range(8):
                                    E.op("pe", lambda e: e.matmul(pz[:, 0:n].rearrange("p (t q) -> p t q", t=nt), lhsT=w1b[:, wpar, kc, fc * 256 + off:fc * 256 + 256:2], rhs=h2T[:, t0:t0 + nt, kc, :],
                                                                  start=(kc == 0), stop=(kc == 7)), reads=hk + [("w1b", wpar, kc)], writes=[PN[id(pz)]])
                            E.op("dve", lambda e: e.tensor_scalar(out=gc[:, s_, 0:n], in0=pg[:, 0:n], scalar1=b1t[:, wpar, fc, 0:1], scalar2=7.0, op0=ALU.add, op1=ALU.min),
                                 reads=[PN[id(pg)], ("b1t", wpar)], writes=[("gc", s_)])
                            E.op("act", lambda e: e.activation(out=sgm[:, s_, 0:n], in_=gc[:, s_, 0:n], func=AF.Sigmoid, scale=1.702), reads=[("gc", s_)], writes=[("sgm", s_), "h2"])
                            E.op("dve", lambda e: e.tensor_scalar(out=lc[:, s_, 0:n], in0=pl_[:, 0:n], scalar1=b1t[:, wpar, fc, 1:2], scalar2=-6.0, op0=ALU.add, op1=ALU.max),
                                 reads=[PN[id(pl_)], ("b1t", wpar)], writes=[("lc", s_)])
                            E.op("dve", lambda e: e.tensor_tensor(out=gc[:, s_, 0:n], in0=gc[:, s_, 0:n], in1=sgm[:, s_, 0:n], op=ALU.mult), reads=[("gc", s_), ("sgm", s_), "h2"], writes=[("gc", s_)])
                            E.op("dve", lambda e: e.scalar_tensor_tensor(out=aT[:, ap, fc, 0:n], in0=lc[:, s_, 0:n], scalar=8.0, in1=gc[:, s_, 0:n], op0=ALU.min, op1=ALU.mult),
                                 reads=[("lc", s_), ("gc", s_)], writes=[("aT", ap)])

                    def Dn(it_i):
                        ex_i, gi = items[it_i]
                        wpar = ex_i % 2
                        ap = it_i % 2
                        t0, nt = groups[gi]
                        for t in range(nt):
                            bi = t0 + t
                            for nh in range(2):
                                for kc in range(8):
                                    E.op("pe", lambda e: e.matmul(pW[nh][:], lhsT=aT[:, ap, kc, t * 128:(t + 1) * 128], rhs=w2b[:, wpar, kc, nh * 512:(nh + 1) * 512], start=(kc == 0), stop=(kc == 7)),
                                         reads=[("aT", ap), ("w2b", wpar, kc)], writes=[("pW", nh)])
                                E.op("dve", lambda e: e.scalar_tensor_tensor(out=acc[:, bi, nh * 512:(nh + 1) * 512], in0=pW[nh][:], scalar=Gt[:, bi, ex_i:ex_i + 1],
                                                                              in1=acc[:, bi, nh * 512:(nh + 1) * 512], op0=ALU.mult, op1=ALU.add),
                                     reads=[("pW", nh), ("G", bi), ("acc", bi)], writes=[("acc", bi)])

                    if nexp > 0:
                        load_w(0, 0)
                        U(0)
                    for it_i in range(len(items)):
                        ex_i, gi = items[it_i]
                        if gi == 0 and ex_i + 1 < nexp:
                            load_w(ex_i + 1, 1 - ex_i % 2)
                        if it_i + 1 < len(items):
                            U(it_i + 1)
                        Dn(it_i)
                    chk(9)
                    for bi, (srcT, rtile, which, dstT, drow) in enumerate(blk):
                        mT = modT[:, which, :, :]
                        E.dma("sp", xt[:], srcT[rtile * 128:(rtile + 1) * 128, :], writes=["xt"])
                        E.op("dve", lambda e: e.tensor_tensor(out=tmpf[:], in0=acc[:, bi, :], in1=mT[:, 2, :], op=ALU.mult), reads=[("acc", bi), "modT"], writes=["tmpf"])
                        E.op("pool", lambda e: e.tensor_tensor(out=tmpf[:], in0=tmpf[:], in1=xt[:], op=ALU.add), reads=["tmpf", "xt"], writes=["tmpf"])
                        if last:
                            E.op("pool", lambda e: e.memset(ssq[:], 0.0), writes=["ssq"])
                            E.op("act", lambda e: e.activation(out=h2[:], in_=tmpf[:], func=AF.Square, accum_out=ssq[:]), reads=["tmpf"], writes=["h2", "ssq"])
                            E.op("act", lambda e: e.activation(out=rt[:], in_=ssq[:], func=AF.Sqrt, scale=1.0 / D, bias=EPS), reads=["ssq"], writes=["rt"])
                            E.op("dve", lambda e: e.reciprocal(out=rt[:], in_=rt[:]), reads=["rt"], writes=["rt"])
                            E.op("dve", lambda e: e.scalar_tensor_tensor(out=h2[:], in0=tmpf[:], scalar=rt[:, 0:1], in1=fg[:], op0=ALU.mult, op1=ALU.mult),
                                 reads=["tmpf", "rt", "fg"], writes=["h2"])
                            E.dma("sp", dstT[drow * 128:(drow + 1) * 128, :], h2[:], reads=["h2"], writes=[("dram", id(dstT), drow)])
                        else:
                            E.dma("sp", dstT[drow * 128:(drow + 1) * 128, :], tmpf[:], reads=["tmpf"], writes=[("dram", id(dstT), drow)])
    except _Stop:
        pass
    E.dead = False
    E.barrier()
    es.close()
    return E


_CACHE = {}


def _get_nc():
    if "nc" not in _CACHE:
        nc0 = bass.Bass("TRN2", target_bir_lowering=False)
        E0 = build(nc0)
        nc = bass.Bass("TRN2", target_bir_lowering=False)
        build(nc, needed=E0.collected)
        _CACHE["nc"] = nc
    return _CACHE["nc"]


def _core_inputs(inputs):
    f32 = np.float32
    x = np.asarray(inputs["x"], f32); c = np.asarray(inputs["c"], f32)
    ctx = np.asarray(inputs["ctx"], f32); c_ctx = np.asarray(inputs["c_ctx"], f32)
    S = x.shape[1]
    shared = {k: np.ascontiguousarray(np.asarray(inputs[k], f32)) for k in (
        "norm1_g", "norm2_g", "ada_w", "ada_b", "w_in", "attn_sink", "sgu_ws", "sgu_b", "sgu_ln_g", "sgu_ln_b",
        "pool_w", "pool_scale", "conv_w", "w_out", "router_w", "router_b", "exp_w1", "exp_b1", "exp_w2", "exp_b2", "final_g")}
    jj = np.arange(128)
    masks = np.stack([(jj[:, None] >= jj[None, :]), (jj[:, None] <= jj[None, :])]).astype(f32)
    inv = (np.float32(10000.0) ** (-np.arange(16, dtype=f32) / np.float32(16))).astype(f32)
    wins = (2, 4, 8, 16)
    maps = []
    for core in range(8):
        b, r = core // 4, core % 4
        s = r * 4096
        xs = np.zeros((NT * 128, D), f32)
        lo, hi = s - 256, s + 4096 + 256
        a, e = max(lo, 0), min(hi, S)
        xs[a - lo:e - lo] = x[b, a:e]
        pos = (lo + np.arange(NT * 128)).astype(np.int64)
        row = (pos // 64).astype(f32); col = (pos % 64).astype(f32)
        ang = np.concatenate([row[:, None] * inv[None, :], col[:, None] * inv[None, :]], axis=1).astype(f32)
        pidx = (np.arange(128) % 64) % 32
        rope = np.stack([np.cos(ang).astype(f32)[:, pidx].T, np.sin(ang).astype(f32)[:, pidx].T]).astype(f32)
        flags = np.zeros((128, 4), f32)
        flags[:, 1] = 1.0 if r > 0 else 0.0
        flags[:, 2] = 1.0 if r < 3 else 0.0
        ptab = np.zeros((128, 5, 2, 128), f32)
        for p in range(128):
            for cc in range(2):
                w = wins[2 * cc + p // 64]
                ptab[p, 0, cc, :] = 1.0 / w
                for idx, (t0, N) in ((1, (s, S)), (2, (s + 31 * 128, S)), (3, (0, 256)), (4, (128, 256))):
                    t = t0 + np.arange(128)
                    cnt = np.clip(t + w - w // 2, 0, N) - np.clip(t - w // 2, 0, N)
                    ptab[p, idx, cc, :] = 1.0 / cnt.astype(f32)
        cvec = np.concatenate([c[b].reshape(8, 128).T, c_ctx.reshape(8, 128).T], axis=1).astype(f32)
        m = dict(shared)
        m.update(xs=xs, ctxb=np.ascontiguousarray(ctx[b]), cvec=np.ascontiguousarray(cvec), rope=np.ascontiguousarray(rope),
                 masks=masks, flags=flags, ptab=ptab)
        maps.append(m)
    return maps


def kernel(**inputs):
    nc = _get_nc()
    maps = _core_inputs(inputs)
    res = run_bass_kernel_spmd(nc, maps, core_ids=list(range(8)))
    x = inputs["x"]
    out = np.zeros(x.shape, np.float32)
    for core in range(8):
        b, r = core // 4, core % 4
        out[b, r * 4096:(r + 1) * 4096, :] = res.results[core]["y"]
    return out
```
